# Optimizing a Trainium2 kernel written in Bass

```python
import jax, jax.numpy as jnp
from jax import lax
import numpy as np

D_MODEL = 1024
BATCH = 8
SEQ = 2048
DEPTH = 2

N_HEADS_A = 8
HEAD_DIM_A = 64
ROPE_DIM = HEAD_DIM_A // 4
NOPE_DIM = HEAD_DIM_A - ROPE_DIM
KV_LATENT = 128
N_IDX_HEADS = 8
IDX_DIM = 64
TOPK_MAX = 256
Q_BLOCK = 128
ROPE_THETA = 500000.0
WIDTH_A = N_HEADS_A * HEAD_DIM_A
N_GROUPS_B = 8
GROUP_DIM_B = 64
WIDTH_B = N_GROUPS_B * GROUP_DIM_B
CHUNK = 128
D_FF = -(-8 * D_MODEL // (3 * 256)) * 256
EPS = 1e-6

IN_SIZES = [WIDTH_A,
            KV_LATENT,
            ROPE_DIM,
            N_IDX_HEADS * IDX_DIM,
            IDX_DIM,
            N_IDX_HEADS,
            2 * WIDTH_B,
            2 * D_MODEL]
D_IN = sum(IN_SIZES)
SPLITS = tuple(int(v) for v in np.cumsum(IN_SIZES)[:-1])

kernel_name = "hybrid_dsa_gmlp_gated_block"


def rms_norm(x, g):
    xf = x.astype(jnp.float32)
    y = xf * lax.rsqrt(jnp.mean(xf * xf, axis=-1, keepdims=True) + EPS)
    return (y * g.astype(jnp.float32)).astype(x.dtype)


def layer_norm(x, g, b):
    xf = x.astype(jnp.float32)
    mu = jnp.mean(xf, axis=-1, keepdims=True)
    var = jnp.mean(jnp.square(xf - mu), axis=-1, keepdims=True)
    y = (xf - mu) * lax.rsqrt(var + 1e-5)
    return (y * g.astype(jnp.float32) + b.astype(jnp.float32)).astype(x.dtype)


def rope_tables(positions):
    inv = ROPE_THETA ** (-jnp.arange(0, ROPE_DIM, 2, dtype=jnp.float32) / ROPE_DIM)
    ang = positions.astype(jnp.float32)[..., None] * inv
    return jnp.cos(ang), jnp.sin(ang)


def partial_rope(x, cos, sin):
    extra = x.ndim - cos.ndim
    shape = cos.shape[:2] + (1,) * extra + cos.shape[2:]
    c, s = cos.reshape(shape), sin.reshape(shape)
    xr = x[..., :ROPE_DIM].astype(jnp.float32)
    x1, x2 = xr[..., :ROPE_DIM // 2], xr[..., ROPE_DIM // 2:]
    rot = jnp.concatenate([x1 * c - x2 * s, x2 * c + x1 * s], axis=-1).astype(x.dtype)
    return jnp.concatenate([rot, x[..., ROPE_DIM:]], axis=-1)


def dsa_mixer(q, c_kv, k_rope, q_idx, k_idx, w_idx, w_uk, w_uv, cos, sin):
    B, S = q.shape[:2]
    top_k = min(TOPK_MAX, S // 4)
    nb = S // Q_BLOCK
    q = partial_rope(q.reshape(B, S, N_HEADS_A, HEAD_DIM_A), cos, sin)
    q_rope, q_nope = q[..., :ROPE_DIM], q[..., ROPE_DIM:]
    q_lat = jnp.einsum('bshn,hnc->bshc', q_nope, w_uk)
    k_rope = partial_rope(k_rope, cos, sin)
    q_idx = partial_rope(q_idx.reshape(B, S, N_IDX_HEADS, IDX_DIM), cos, sin)
    k_idx = partial_rope(k_idx, cos, sin)
    w_idx = w_idx * (N_IDX_HEADS ** -0.5)
    key_pos = jnp.arange(S)
    gather = jax.vmap(lambda a, i: a[i])

    def to_blocks(a):
        return jnp.moveaxis(a.reshape((B, nb, Q_BLOCK) + a.shape[2:]), 1, 0)

    def block(args):
        ql, qr, qi, wi, qpos = args
        isc = jnp.einsum('bthd,bsd->bths', qi, k_idx,
                         preferred_element_type=jnp.float32) * (IDX_DIM ** -0.5)
        isc = jnp.einsum('bths,bth->bts', jax.nn.relu(isc), wi.astype(jnp.float32))
        causal = key_pos[None, :] <= qpos[:, None]
        isc = jnp.where(causal[None], isc, -jnp.inf)
        _, sel = lax.top_k(isc, top_k)
        valid = sel <= qpos[None, :, None]
        c_sel = gather(c_kv, sel)
        kr_sel = gather(k_rope, sel)
        sc = (jnp.einsum('bthc,btkc->bthk', ql, c_sel, preferred_element_type=jnp.float32)
              + jnp.einsum('bthr,btkr->bthk', qr, kr_sel, preferred_element_type=jnp.float32)
              ) * (HEAD_DIM_A ** -0.5)
        sc = jnp.where(valid[:, :, None, :], sc, -jnp.inf)
        p = jax.nn.softmax(sc, axis=-1).astype(c_sel.dtype)
        o_lat = jnp.einsum('bthk,btkc->bthc', p, c_sel)
        return jnp.einsum('bthc,hcd->bthd', o_lat, w_uv)

    out = lax.map(block, (to_blocks(q_lat), to_blocks(q_rope), to_blocks(q_idx),
                          to_blocks(w_idx), key_pos.reshape(nb, Q_BLOCK)))
    return jnp.moveaxis(out, 0, 1).reshape(B, S, WIDTH_A)


def chunked_gmlp(uv, ln_g, ln_b, w_s, b_s):
    B, S = uv.shape[:2]
    uv = jax.nn.gelu(uv)
    u, v = jnp.split(uv, 2, axis=-1)
    v = layer_norm(v, ln_g, ln_b)
    nc = S // CHUNK
    v = v.reshape(B, nc, CHUNK, N_GROUPS_B, GROUP_DIM_B)
    mask = jnp.tril(jnp.ones((CHUNK, CHUNK), dtype=bool))
    ws = jnp.where(mask[None], w_s, 0.0)
    s = jnp.einsum('gts,bcsgd->bctgd', ws, v) + b_s.T[None, None, :, :, None]
    return u * s.reshape(B, S, WIDTH_B)


def setup_inputs(seed: int = 0) -> dict:
    key = jax.random.key(seed)
    ks = jax.random.split(key, 20)
    f32 = jnp.float32

    def nrm(k, shape, fan_in):
        return jax.random.normal(k, shape, f32) * (fan_in ** -0.5)

    def gain(k, shape):
        return 1.0 + 0.02 * jax.random.normal(k, shape, f32)

    L = DEPTH
    return {
        "x": jax.random.normal(ks[0], (BATCH, SEQ, D_MODEL), f32),
        "positions": jnp.broadcast_to(jnp.arange(SEQ, dtype=jnp.int32), (BATCH, SEQ)),
        "norm_mix": gain(ks[1], (L, D_MODEL)),
        "w_in": nrm(ks[2], (L, D_MODEL, D_IN), D_MODEL),
        "kv_norm": gain(ks[3], (L, KV_LATENT)),
        "w_uk": nrm(ks[4], (L, N_HEADS_A, NOPE_DIM, KV_LATENT), NOPE_DIM),
        "w_uv": nrm(ks[5], (L, N_HEADS_A, KV_LATENT, HEAD_DIM_A), KV_LATENT),
        "ln_v_g": gain(ks[6], (L, WIDTH_B)),
        "ln_v_b": 0.02 * jax.random.normal(ks[7], (L, WIDTH_B), f32),
        "w_s": nrm(ks[8], (L, N_GROUPS_B, CHUNK, CHUNK), CHUNK),
        "b_s": 1.0 + 0.01 * jax.random.normal(ks[9], (L, N_GROUPS_B, CHUNK), f32),
        "w_proj_a": nrm(ks[10], (L, WIDTH_A, D_MODEL), WIDTH_A),
        "w_proj_b": nrm(ks[11], (L, WIDTH_B, D_MODEL), WIDTH_B),
        "w_out": nrm(ks[12], (L, D_MODEL, D_MODEL), D_MODEL),
        "norm_ffn": gain(ks[13], (L, D_MODEL)),
        "w_gate": nrm(ks[14], (L, D_MODEL, D_FF), D_MODEL),
        "w_up": nrm(ks[15], (L, D_MODEL, D_FF), D_MODEL),
        "w_down": nrm(ks[16], (L, D_FF, D_MODEL), D_FF),
        "final_norm": gain(ks[17], (D_MODEL,)),
    }


def reference(x, positions, norm_mix, w_in, kv_norm, w_uk, w_uv, ln_v_g, ln_v_b, w_s, b_s,
              w_proj_a, w_proj_b, w_out, norm_ffn, w_gate, w_up, w_down, final_norm):
    cos, sin = rope_tables(positions)
    for l in range(DEPTH):
        h = rms_norm(x, norm_mix[l])
        z = h @ w_in[l]
        q, c_kv, k_rope, q_idx, k_idx, w_idx, uv, gates = jnp.split(z, SPLITS, axis=-1)
        c_kv = rms_norm(c_kv, kv_norm[l])
        y_a = dsa_mixer(q, c_kv, k_rope, q_idx, k_idx, w_idx, w_uk[l], w_uv[l], cos, sin)
        y_b = chunked_gmlp(uv, ln_v_g[l], ln_v_b[l], w_s[l], b_s[l])
        g_a, g_b = jnp.split(jax.nn.sigmoid(gates), 2, axis=-1)
        merged = g_a * (y_a @ w_proj_a[l]) + g_b * (y_b @ w_proj_b[l])
        x = x + merged @ w_out[l]
        h = rms_norm(x, norm_ffn[l])
        x = x + (jax.nn.silu(h @ w_gate[l]) * (h @ w_up[l])) @ w_down[l]
    return rms_norm(x, final_norm)
```

```python
import contextlib
import numpy as np
import ml_dtypes
import concourse.bass as bass
import concourse.mybir as mybir
from concourse.bass_utils import run_bass_kernel_spmd

F32 = mybir.dt.float32
BF16 = mybir.dt.bfloat16
I32 = mybir.dt.int32
ALU = mybir.AluOpType
AF = mybir.ActivationFunctionType
AX = mybir.AxisListType

D = 1024
S = 2048
L = 2
NT = 16
TB = 512
NB = S // TB
DFF = 2816
NFC = DFF // 128
TOPK = 256
THETA = 500000.0
NEG = -30000.0

OFF_Q, OFF_QI, OFF_KX, OFF_CKV, OFF_KR = 0, 512, 1024, 1152, 1280
OFF_U, OFF_GA, OFF_GB, OFF_V, OFF_WI = 1296, 1808, 2832, 3856, 4368
WIN_COLS = 4376


class Sched:
    def __init__(self, nc):
        self.nc = nc
        self.ops = []

    def add(self, eng, fn, r=(), w=(), dma=False):
        self.ops.append({"eng": eng, "fn": fn, "r": list(r), "w": list(w), "dma": dma})

    def mm(self, out, lhsT, rhs, start, stop, r, w):
        self.add("pe", lambda e: e.matmul(out, lhsT, rhs, start=start, stop=stop, skip_group_check=True), r, w)

    def tr(self, out, in_, ident, r, w):
        self.add("pe", lambda e: e.transpose(out, in_, ident), r, w)

    def act(self, out, in_, func, r, w, bias=None, scale=None, accum_out=None):
        kw = {}
        if bias is not None:
            kw["bias"] = bias
        if scale is not None:
            kw["scale"] = scale
        if accum_out is not None:
            kw["accum_out"] = accum_out
        self.add("act", lambda e: e.activation(out, in_, func, **kw), r, w)

    def tt(self, eng, out, in0, in1, op, r, w):
        self.add(eng, lambda e: e.tensor_tensor(out, in0, in1, op), r, w)

    def ts(self, eng, out, in0, s1, s2, op0, op1, r, w, accum_out=None):
        if accum_out is not None:
            self.add(eng, lambda e: e.tensor_scalar(out, in0, s1, s2, op0, op1, accum_out=accum_out), r, w)
        elif op1 is None:
            self.add(eng, lambda e: e.tensor_scalar(out, in0, s1, None, op0), r, w)
        else:
            self.add(eng, lambda e: e.tensor_scalar(out, in0, s1, s2, op0, op1), r, w)

    def stt(self, eng, out, in0, scalar, in1, op0, op1, r, w):
        self.add(eng, lambda e: e.scalar_tensor_tensor(out, in0, scalar, in1, op0, op1), r, w)

    def copy(self, eng, out, in_, r, w):
        if eng == "act":
            self.add(eng, lambda e: e.copy(out, in_), r, w)
        else:
            self.add(eng, lambda e: e.tensor_copy(out, in_), r, w)

    def memset(self, eng, ap, val, w):
        self.add(eng, lambda e: e.memset(ap, val), (), w)

    def dma(self, eng, out, in_, r, w):
        self.add(eng, lambda e: e.dma_start(out=out, in_=in_), r, w, dma=True)

    def emit(self, stack, final_keys):
        nc = self.nc
        ops = self.ops
        engs = ["pe", "act", "dve", "pool", "sp"]
        pos_ctr = {e: 0 for e in engs}
        last_w = {}
        readers = {}
        for i, op in enumerate(ops):
            op["pos"] = pos_ctr[op["eng"]]
            pos_ctr[op["eng"]] += 1
            deps = {}

            def add_dep(j, kind):
                if j == i:
                    return
                prev = deps.get(j)
                if prev is None or (prev == "war" and kind != "war"):
                    deps[j] = kind

            for k in op["r"]:
                if k in last_w:
                    add_dep(last_w[k], "raw")
            for k in op["w"]:
                if k in last_w:
                    add_dep(last_w[k], "waw")
                for j in readers.get(k, {}).values():
                    add_dep(j, "war")
            for k in op["r"]:
                readers.setdefault(k, {})[op["eng"]] = i
            for k in op["w"]:
                last_w[k] = i
                readers[k] = {}
            op["deps"] = deps
            op["signal"] = False

        def needs_sync(j, i, kind):
            oj, oi = ops[j], ops[i]
            if oj["dma"]:
                return True
            if oj["eng"] != oi["eng"]:
                return True
            if oi["eng"] == "pe":
                return False
            if oi["eng"] == "pool":
                return True
            if kind == "war":
                return False
            return (oi["pos"] - oj["pos"]) <= 3

        for i, op in enumerate(ops):
            op["sdeps"] = [j for j, kind in op["deps"].items() if needs_sync(j, i, kind)]
            for j in op["sdeps"]:
                ops[j]["signal"] = True
        final_ops = [last_w[k] for k in final_keys]
        for j in final_ops:
            ops[j]["signal"] = True

        esem = {e: stack.enter_context(nc.semaphore("sem_" + e)) for e in engs}
        NS = 12
        dsem = [stack.enter_context(nc.semaphore("dsem%d" % k)) for k in range(NS)]
        ecount = {e: 0 for e in engs}
        dcount = 0
        for op in ops:
            if op["dma"]:
                k = dcount
                dcount += 1
                op["sig"] = (dsem[k % NS], 16 * (k // NS + 1))
                op["dprev"] = (dsem[k % NS], 16 * (k // NS)) if k >= NS else None
            elif op["signal"]:
                ecount[op["eng"]] += 1
                op["sig"] = (esem[op["eng"]], ecount[op["eng"]])
        self.stats = {e: pos_ctr[e] for e in engs}
        self.stats["signals"] = dict(ecount)

        def run_engine(ename, eng):
            waited = {}
            for op in ops:
                if op["eng"] != ename:
                    continue
                waits = [ops[j]["sig"] for j in op["sdeps"]]
                if op["dma"] and op["dprev"] is not None:
                    waits.append(op["dprev"])
                best = {}
                for sem, val in waits:
                    key = id(sem)
                    if key not in best or best[key][1] < val:
                        best[key] = (sem, val)
                for key, (sem, val) in best.items():
                    if waited.get(key, 0) >= val:
                        continue
                    eng.wait_ge(sem, val)
                    waited[key] = val
                if op.get("pad"):
                    if ename == "dve":
                        eng.memset(self.dummy[:, 0:1], 0.0)
                    elif ename == "act":
                        eng.memzero(self.dummy[:, 1:2])
                ins = op["fn"](eng)
                if op["dma"]:
                    ins.then_inc(op["sig"][0], 16)
                elif op["signal"]:
                    ins.then_inc(op["sig"][0], 1)
            if ename == "sp":
                for j in final_ops:
                    sem, val = ops[j]["sig"]
                    eng.wait_ge(sem, val)

        with nc.Block() as block:
            @block.tensor
            def _(e):
                run_engine("pe", e)

            @block.scalar
            def _(e):
                run_engine("act", e)

            @block.vector
            def _(e):
                run_engine("dve", e)

            @block.gpsimd
            def _(e):
                run_engine("pool", e)

            @block.sync
            def _(e):
                run_engine("sp", e)


class _Stop(Exception):
    pass


def build_nc(n_layers=L, dbg=None, stop=None):
    nc = bass.Bass("TRN2", target_bir_lowering=False)
    stack = contextlib.ExitStack()
    Sd = Sched(nc)

    def din(name, shape, dt=F32):
        return nc.dram_tensor(name, list(shape), dt, kind="ExternalInput").ap()

    x_d = din("x", [S, D])
    pos_d = din("pos", [1, S], I32)
    win_d = din("w_in_r", [L, D, WIN_COLS])
    wuk_d = din("w_ukT", [L, 128, 8 * 64])
    wuv_d = din("w_uvr", [L, 128, 8 * 64])
    wsT_d = din("w_sT", [L, 128, 8 * 128])
    bs_d = din("b_sr", [L, 1, 8 * 128])
    lng_d = din("ln_g", [L, 1, 512])
    lnb_d = din("ln_b", [L, 1, 512])
    wpa_d = din("w_pa", [L, 512, D])
    wpb_d = din("w_pb", [L, 512, D])
    wout_d = din("w_out", [L, D, D])
    wg_d = din("w_gate", [L, D, DFF])
    wu_d = din("w_up", [L, D, DFF])
    wd_d = din("w_down", [L, DFF, D])
    gains_d = din("gains", [128, 48])
    cf32_d = din("cf32", [128, 3 * 128])
    cbf_d = din("cbf", [128, 5 * 128], BF16)
    out_d = nc.dram_tensor("out", [S, D], F32, kind="ExternalOutput").ap()

    def sb(name, shape, dt):
        return stack.enter_context(nc.sbuf_tensor("s_" + name, list(shape), dt))

    def ps(name, shape, dt):
        return stack.enter_context(nc.psum_tensor(name, list(shape), dt))

    xT = sb("xT", [128, 8, S], F32)
    hT = sb("hT", [128, 8, TB], BF16)
    kT_all = sb("kT_all", [128, 4, S], BF16)
    Vt = sb("Vt", [128, NT, 8 * 65], BF16)
    kidx2 = sb("kidx2", [128, S], BF16)
    ckvn = sb("ckvn", [128, TB], BF16)
    kropeT = sb("kropeT", [128, TB], BF16)
    Zt = sb("Zt", [128, 512], BF16)
    bs128 = sb("bs128", [128, 8 * 128], BF16)
    one128 = sb("one128", [128, 64], BF16)
    rden = sb("rden", [128, 8], F32)
    qv = sb("qv", [128, 8, TB], BF16)
    qT = qv[:, 0:4, :]
    vln = qv[:, 4:8, :]
    mergedT = qv
    qiT = sb("qiT", [128, 4, TB], BF16)
    yaT = qiT
    uT = sb("uT", [128, 4, TB], BF16)
    ybT = uT
    widx = sb("widx", [128, 4, 8], F32)
    arena = sb("arena", [128, NFC * TB], BF16)
    hid = arena[:].rearrange("p (a b) -> p a b", a=NFC)
    isc = arena[:, 0:4096].bitcast(F32)
    maskb = arena[:, 4096:6144]
    maskT = arena[:, 6144:8192].rearrange("p (j t) -> p j t", j=NT)
    Pexp = [arena[:, 8192:9216].rearrange("p (h t) -> p h t", h=8)]
    PT = [arena[:, 9216:10240].rearrange("p (h t) -> p h t", h=8)]
    zb = arena[:, 10240:10752]
    ya = arena[:, 10752:11264]
    Rh = [sb("Rh%d" % k, [128, 512], BF16) for k in range(2)]
    Dg = sb("Dg", [128, 8, 128], BF16)
    NF = 6
    fs = [sb("fs%d" % k, [128, 512], F32) for k in range(NF)]
    NW = 3
    wt = [sb("wt%d" % k, [128, 8 * 520], BF16) for k in range(NW)]
    Ctab = sb("Ctab", [128, S], BF16)
    Stab = sb("Stab", [128, S], BF16)
    wsTm = sb("wsTm", [128, 8 * 128], BF16)
    wukT = sb("wukT", [128, 8 * 64], BF16)
    wuvr = sb("wuvr", [128, 8 * 64], BF16)
    gains = sb("gains", [128, 48], F32)
    cf32 = sb("cf32", [128, 3 * 128], F32)
    cbf = sb("cbf", [128, 5 * 128], BF16)
    small = sb("small", [128, 64], F32)
    Sd.dummy = small[:, 32:34]
    stats = sb("stats", [128, 16], F32)

    ident_f = cf32[:, 0:128]
    tri_neg = cf32[:, 128:256]
    ones_f = cf32[:, 256:384]
    ident_b = cbf[:, 0:128]
    triT = cbf[:, 128:256]
    Rrot = cbf[:, 256:384]
    E16 = cbf[:, 384:448]
    ones_b = cbf[:, 512:640]

    pmain = ps("pmain", [128, 4 * 512], F32)
    pacc_t = ps("pacc_t", [128, 512], F32)
    pbf = ps("pbf", [128, 1024], BF16)
    pO = ps("pO", [128, 2 * 512], F32)

    state = {"pb": 0, "fs": 0, "wt": 0, "rh": 0, "pbf": 0, "pp": 0}

    def pbank(n=1):
        b = state["pb"]
        if b + n > 4:
            b = 0
        state["pb"] = (b + n) % 4
        return pmain[:, b * 512:(b + n) * 512], [("ps", b + k) for k in range(n)]

    def pbfhalf():
        h = state["pbf"]
        state["pbf"] = 1 - h
        return pbf[:, h * 512:(h + 1) * 512], [("pbf", 0)]

    def fscr():
        k = state["fs"]
        state["fs"] = (k + 1) % NF
        return fs[k], ("fs", k)

    def wslot():
        k = state["wt"]
        state["wt"] = (k + 1) % NW
        return wt[k], ("wt", k)

    def rhslot():
        k = state["rh"]
        state["rh"] = (k + 1) % 2
        return Rh[k], ("rh", k)

    def ppslot():
        k = 0
        return Pexp[k], PT[k], ("pexp", k), ("pt", k)

    dbg_outs = {}

    def dump(name, ap, shape, dt, keys):
        if dbg is None or name not in dbg:
            return
        t = nc.dram_tensor("dbg_" + name, list(shape), dt, kind="ExternalOutput").ap()
        Sd.dma("sp", t, ap, keys, [("dbg", name)])
        dbg_outs[name] = ("dbg_" + name, ("dbg", name))

    Sd.dma("sp", gains[:], gains_d, [], ["gains"])
    Sd.dma("sp", cf32[:], cf32_d, [], ["cf32"])
    Sd.dma("sp", cbf[:], cbf_d, [], ["cbf"])
    Sd.memset("dve", Zt[:], 0.0, ["Zt"])
    Sd.memset("dve", kropeT[:], 0.0, ["kropeT"])
    Sd.memset("dve", bs128[:], 0.0, ["bs128"])
    Sd.memset("dve", one128[:], 0.0, ["one128"])
    Sd.memset("dve", one128[0:1, :], 1.0, ["one128"])

    posi = arena[:, 4096:8192].bitcast(I32)
    Sd.dma("sp", posi[:], pos_d.partition_broadcast(128), [], ["posi"])
    invf = gains[:, 42:43]
    PI = float(np.pi)
    MAGIC = 12582912.0
    HI = 6.28125
    LO = float(2 * np.pi - 6.28125)

    def sin_of(dst, ang, angk, dkey):
        t, tk = fs[2], ("fs", 2)
        r, rk_ = fs[3], ("fs", 3)
        a, ak = fs[4], ("fs", 4)
        c, ck = fs[5], ("fs", 5)
        Sd.ts("dve", t[:], ang[:], 1.0 / (2 * PI), MAGIC, ALU.mult, ALU.add, [angk], [tk])
        Sd.ts("dve", r[:], t[:], MAGIC, None, ALU.subtract, None, [tk], [rk_])
        Sd.stt("dve", a[:], r[:], -HI, ang[:], ALU.mult, ALU.add, [rk_, angk], [ak])
        Sd.stt("dve", c[:], r[:], -LO, a[:], ALU.mult, ALU.add, [rk_, ak], [ck])
        Sd.ts("dve", a[:], c[:], PI, -PI, ALU.min, ALU.max, [ck], [ak])
        Sd.act(dst, a[:], AF.Sin, [ak], [dkey])

    for half in range(4):
        sl = slice(half * 512, (half + 1) * 512)
        a0, k0 = fs[0], ("fs", 0)
        a1, k1 = fs[1], ("fs", 1)
        Sd.copy("dve", a0[:], posi[:, sl], ["posi"], [k0])
        Sd.ts("dve", a1[:], a0[:], invf, None, ALU.mult, None, [k0, "gains"], [k1])
        sin_of(Stab[:, sl], a1, k1, ("Stab", half))
        Sd.ts("dve", a0[:], a1[:], 0.5 * PI, None, ALU.add, None, [k1], [k0])
        sin_of(Ctab[:, sl], a0, k0, ("Ctab", half))

    hidflat = arena
    xin = [hidflat[:, 0:2048].bitcast(F32), hidflat[:, 2048:4096].bitcast(F32)]
    for i in range(NT):
        st = xin[i % 2]
        sk = ("xin", i % 2)
        Sd.dma("sp", st, x_d[i * 128:(i + 1) * 128, :], [], [sk])
        for half in range(2):
            pt, pk = pbank()
            for c in range(4):
                kc = half * 4 + c
                Sd.tr(pt[:, c * 128:(c + 1) * 128], st[:, kc * 128:(kc + 1) * 128], ident_f, [sk, "cf32"], pk)
            dst = xT[:, half * 4:(half + 1) * 4, i * 128:(i + 1) * 128]
            src = pt.rearrange("p (c t) -> p c t", c=4)
            Sd.copy("dve" if half == 0 else "act", dst, src, pk, [("xT", i // 4)])

    def rmsnorm_block(b, gcol0, dst, dst_key, extra_scale_keys=()):
        blk = slice(b * TB, (b + 1) * TB)
        pt, pk = pbank()
        for kc in range(8):
            sq, sqk = rhslot()
            Sd.act(sq[:], xT[:, kc, blk], AF.Square, [("xT", b)], [sqk])
            Sd.mm(pt, ones_b, sq[:], kc == 0, kc == 7, [sqk, "cbf"], pk)
        ms, msk = fscr()
        Sd.ts("dve", ms[:], pt, 1.0 / D, 1e-6, ALU.mult, ALU.add, pk, [msk])
        rB, rk = fscr()
        Sd.tt("pool", rB[:], ms[:], small[:, 0:1].to_broadcast([128, TB]), ALU.pow, [msk, "small"], [rk])
        for kc in range(8):
            Sd.stt("dve", dst[:, kc, :], xT[:, kc, blk], gains[:, gcol0 + kc:gcol0 + kc + 1], rB[:],
                   ALU.mult, ALU.mult, [("xT", b), rk, "gains"], [dst_key])

    Sd.memset("dve", small[:, 0:1], -0.5, ["small"])

    scratch = {}
    stage = [(kT_all[:].rearrange("p a b -> p (a b)").bitcast(F32), "stage0"),
             (Vt[:].rearrange("p a b -> p (a b)").bitcast(F32), "stage1")]
    pstate = {"i": 0}
    cast_engs = ["dve", "act", "pool"]

    def prologue_chunk(l, name, src_ap):
        kcn, ncol = src_ap.shape[1], src_ap.shape[2]
        n = kcn * ncol
        sc = nc.dram_tensor("sc_%d_%s" % (l, name), [128, n], BF16).ap()
        i = pstate["i"]
        pstate["i"] += 1
        st, stk = stage[i % 2]
        Sd.dma("sp", st[:, 0:n].rearrange("p (k n) -> p k n", k=kcn), src_ap, [], [stk])
        w, wk = wslot()
        Sd.copy(cast_engs[i % 3], w[:, 0:n], st[:, 0:n], [stk], [wk])
        Sd.dma("sp", sc, w[:, 0:n], [wk], [("sc", l, name)])
        scratch[(l, name)] = (sc, kcn, ncol)

    def load_w(l, name):
        sc, kcn, ncol = scratch[(l, name)]
        w, wk = wslot()
        n = kcn * ncol
        Sd.dma("sp", w[:, 0:n], sc, [("sc", l, name)], [wk])
        return w[:, 0:n].rearrange("p (k n) -> p k n", k=kcn), wk

    for l in range(n_layers):
        winv = win_d[l].rearrange("(kc p) f -> p kc f", p=128)
        prologue_chunk(l, "kx", winv[:, :, OFF_KX:OFF_KX + 272])
        prologue_chunk(l, "q", winv[:, :, OFF_Q:OFF_Q + 512])
        prologue_chunk(l, "qi", winv[:, :, OFF_QI:OFF_QI + 512])
        prologue_chunk(l, "v", winv[:, :, OFF_V:OFF_V + 512])
        prologue_chunk(l, "wi", winv[:, :, OFF_WI:OFF_WI + 8])
        prologue_chunk(l, "u", winv[:, :, OFF_U:OFF_U + 512])
        wpav = wpa_d[l].rearrange("(kc p) f -> p kc f", p=128)
        wpbv = wpb_d[l].rearrange("(kc p) f -> p kc f", p=128)
        wov = wout_d[l].rearrange("(kc p) f -> p kc f", p=128)
        for gh in range(2):
            prologue_chunk(l, "ga%d" % gh, winv[:, :, OFF_GA + gh * 512:OFF_GA + (gh + 1) * 512])
            prologue_chunk(l, "gb%d" % gh, winv[:, :, OFF_GB + gh * 512:OFF_GB + (gh + 1) * 512])
            prologue_chunk(l, "pa%d" % gh, wpav[:, :, gh * 512:(gh + 1) * 512])
            prologue_chunk(l, "pb%d" % gh, wpbv[:, :, gh * 512:(gh + 1) * 512])
            prologue_chunk(l, "wo%d" % gh, wov[:, :, gh * 512:(gh + 1) * 512])
        wgv_ = wg_d[l].rearrange("(kc p) f -> p kc f", p=128)
        wuv2_ = wu_d[l].rearrange("(kc p) f -> p kc f", p=128)
        for j in range(6):
            ncol = 512 if j < 5 else 256
            prologue_chunk(l, "wg%d" % j, wgv_[:, :, j * 512:j * 512 + ncol])
            prologue_chunk(l, "wu%d" % j, wuv2_[:, :, j * 512:j * 512 + ncol])
        wdv = wd_d[l].rearrange("(fc p) d -> p fc d", p=128)
        for fo in range(8):
            prologue_chunk(l, "wd%d" % fo, wdv[:, :, fo * 128:(fo + 1) * 128])
    Sd.memset("pool", Vt[:], 1.0, ["Vt_init", "stage1"])
    if stop == "p0":
        n_layers = 0

    def ckpt(name):
        if stop == name:
            raise _Stop()

    try:
      for l in range(n_layers):
          winv = win_d[l].rearrange("(kc p) f -> p kc f", p=128)
          t0, tk0 = fscr()
          Sd.dma("sp", t0[:], wuk_d[l], [], [tk0])
          Sd.copy("dve", wukT[:], t0[:], [tk0], ["wukT"])
          t1, tk1 = fscr()
          Sd.dma("sp", t1[:], wuv_d[l], [], [tk1])
          Sd.copy("dve", wuvr[:], t1[:], [tk1], ["wuvr"])
          for hf in range(2):
              t2, tk2 = fscr()
              Sd.dma("sp", t2[0:1, :], bs_d[l][:, hf * 512:(hf + 1) * 512], [], [tk2])
              Sd.copy("dve", bs128[0:1, hf * 512:(hf + 1) * 512], t2[0:1, :], [tk2], ["bs128"])
              t3, tk3 = fscr()
              Sd.dma("sp", t3[:], wsT_d[l][:, hf * 512:(hf + 1) * 512], [], [tk3])
              Sd.tt("dve", wsTm[:, hf * 512:(hf + 1) * 512].rearrange("p (g t) -> p g t", g=4),
                    t3[:].rearrange("p (g t) -> p g t", g=4),
                    triT.unsqueeze(1).to_broadcast([128, 4, 128]), ALU.mult, [tk3, "cbf"], ["wsTm"])

          for b in range(NB):
              blk = slice(b * TB, (b + 1) * TB)
              rmsnorm_block(b, l * 8, hT, "hT")
              if l == 0 and b == 0:
                  dump("hT", hT[:], [128, 8, TB], BF16, ["hT"])
              ckpt("p1")

              def proj_fm(wview, wk, c0, M, kcn, rhs_of_kc, rkeys):
                  pt, pk = pbank()
                  for kc in range(kcn):
                      Sd.mm(pt[0:M, :], wview[:, kc, c0:c0 + M], rhs_of_kc(kc), kc == 0, kc == kcn - 1,
                            [wk] + rkeys, pk)
                  return pt, pk

              def rope_evac(pt, pk, M, dst, dkey):
                  Sd.copy("act", zb[0:M, :], pt[0:M, :], pk, ["zb"])
                  zs, zsk = rhslot()
                  Sd.tt("dve", zs[0:M, :], zb[0:M, :], Stab[0:M, blk], ALU.mult, ["zb", ("Stab", b)], [zsk])
                  p2, pk2 = pbank()
                  Sd.mm(p2[0:M, :], Rrot[0:M, 0:M], zs[0:M, :], True, True, [zsk, "cbf"], pk2)
                  zc, zck = fscr()
                  Sd.tt("dve", zc[0:M, :], zb[0:M, :], Ctab[0:M, blk], ALU.mult, ["zb", ("Ctab", b)], [zck])
                  Sd.tt("dve", dst, p2[0:M, :], zc[0:M, :], ALU.add, pk2 + [zck], [dkey])

              hrhs = lambda kc: hT[:, kc, :]
              wv, wk = load_w(l, "kx")
              ckpt("p2w")
              pt, pk = proj_fm(wv, wk, 0, 128, 8, hrhs, ["hT"])
              if stop == "p2x":
                  Sd.copy("act", kidx2[:, blk], pt, pk, [("kidx2", b)])
                  dump("kidx2", kidx2[:, 0:TB], [128, TB], BF16, [("kidx2", 0)])
              ckpt("p2x")
              rope_evac(pt, pk, 128, kidx2[:, blk], ("kidx2", b))
              ckpt("p2a")
              pt, pk = proj_fm(wv, wk, 256, 16, 8, hrhs, ["hT"])
              rope_evac(pt, pk, 16, kropeT[0:16, :], "kropeT")
              ckpt("p2b")
              pt, pk = proj_fm(wv, wk, 128, 128, 8, hrhs, ["hT"])
              sq, sqk = rhslot()
              Sd.act(sq[:], pt, AF.Square, pk, [sqk])
              p2, pk2 = pbank()
              Sd.mm(p2, ones_b, sq[:], True, True, [sqk, "cbf"], pk2)
              ms, msk = fscr()
              Sd.ts("dve", ms[:], p2, 1.0 / 128, 1e-6, ALU.mult, ALU.add, pk2, [msk])
              rB, rk = fscr()
              Sd.tt("pool", rB[:], ms[:], small[:, 0:1].to_broadcast([128, TB]), ALU.pow, [msk, "small"], [rk])
              Sd.stt("dve", ckvn[:], pt, gains[:, 40 + l:41 + l], rB[:], ALU.mult, ALU.mult, pk + [rk, "gains"], ["ckvn"])
              ckpt("p2c")
              for pr in range(4):
                  pt, pk = pbank()
                  for hh in range(2):
                      h = pr * 2 + hh
                      o = pt[hh * 64:(hh + 1) * 64, :]
                      Sd.mm(o, wukT[:, h * 64:(h + 1) * 64], ckvn[:], True, False, ["wukT", "ckvn"], pk)
                      Sd.mm(o, E16, kropeT[:, :], False, True, ["cbf", "kropeT"], pk)
                  Sd.copy("act", kT_all[:, pr, blk], pt, pk, [("kT", b), "stage0"])
              ckpt("p2d")
              for tt_ in range(4):
                  i = b * 4 + tt_
                  pt, pk = pbank()
                  Sd.mm(pt, ckvn[:, tt_ * 128:(tt_ + 1) * 128], wuvr[:], True, True, ["ckvn", "wuvr"], pk)
                  dst = Vt[:, i, :].rearrange("p (h d) -> p h d", h=8)[:, :, 0:64]
                  Sd.copy("act", dst, pt.rearrange("p (h d) -> p h d", h=8), pk + ["Vt_init"], [("Vt", i)])
              if l == 0 and b == 0:
                  dump("kidx2", kidx2[:, 0:TB], [128, TB], BF16, [("kidx2", 0)])
                  dump("kT", kT_all[:, :, 0:TB], [128, 4, TB], BF16, [("kT", 0)])
                  dump("Vt", Vt[:, 0:4, :], [128, 4, 520], BF16, [("Vt", k) for k in range(4)])
              ckpt("p2")

              wv, wk = load_w(l, "q")
              for c in range(4):
                  pt, pk = proj_fm(wv, wk, c * 128, 128, 8, hrhs, ["hT"])
                  rope_evac(pt, pk, 128, qT[:, c, :], "qT")
              wv, wk = load_w(l, "qi")
              for c in range(4):
                  pt, pk = proj_fm(wv, wk, c * 128, 128, 8, hrhs, ["hT"])
                  rope_evac(pt, pk, 128, qiT[:, c, :], "qiT")
              if l == 0 and b == 0:
                  dump("qT", qT[:], [128, 4, TB], BF16, ["qT"])
              ckpt("p4")

              wv, wk = load_w(l, "v")
              wiv, wik = load_w(l, "wi")
              lngb, lngk = arena[:, 8192:9216].bitcast(F32), ("pexp", 0)
              Sd.dma("sp", lngb, lng_d[l].partition_broadcast(128), [], [lngk, ("hid", 16), ("hid", 17)])
              lnbb, lnbk = arena[:, 9216:10240].bitcast(F32), ("pt", 0)
              Sd.dma("sp", lnbb, lnb_d[l].partition_broadcast(128), [], [lnbk, ("hid", 18), ("hid", 19)])
              for tt_ in range(4):
                  tsl = slice(tt_ * 128, (tt_ + 1) * 128)
                  pt, pk = pbank()
                  for kc in range(8):
                      Sd.mm(pt, hT[:, kc, tsl], wv[:, kc, 0:512], kc == 0, kc == 7, ["hT", wk], pk)
                  gv, gk = fscr()
                  Sd.act(gv[:], pt, AF.Gelu_apprx_tanh, pk, [gk])
                  Sd.add("dve", lambda e, gv=gv: e.bn_stats(stats[:, 0:6], gv[:]), [gk], ["stats6"])
                  Sd.add("dve", lambda e: e.bn_aggr(stats[:, 6:8], stats[:, 0:6]), ["stats6"], ["stats2"])
                  Sd.ts("dve", stats[:, 8:9], stats[:, 7:8], 1e-5, None, ALU.add, None, ["stats2"], ["stats_v"])
                  Sd.tt("pool", stats[:, 9:10], stats[:, 8:9], small[:, 0:1], ALU.pow, ["stats_v", "small"], ["stats_r"])
                  nv, nk = fscr()
                  Sd.ts("dve", nv[:], gv[:], stats[:, 6:7], stats[:, 9:10], ALU.subtract, ALU.mult,
                        [gk, "stats2", "stats_r"], [nk])
                  n2, nk2 = fscr()
                  Sd.tt("pool", n2[:], nv[:], lngb, ALU.mult, [nk, lngk], [nk2])
                  Sd.tt("pool", vln[:, tt_, :], n2[:], lnbb, ALU.add, [nk2, lnbk], [("vln", tt_)])
                  pt, pk = pbank()
                  for kc in range(8):
                      Sd.mm(pt[:, 0:8], hT[:, kc, tsl], wiv[:, kc, 0:8], kc == 0, kc == 7, ["hT", wik], pk)
                  Sd.copy("dve", widx[:, tt_, :], pt[:, 0:8], pk, [("widx", tt_)])
              wv, wk = load_w(l, "u")
              for c in range(4):
                  pt, pk = proj_fm(wv, wk, c * 128, 128, 8, hrhs, ["hT"])
                  Sd.act(uT[:, c, :], pt, AF.Gelu_apprx_tanh, pk, ["uT"])
              if l == 0 and b == 0:
                  dump("vln", vln[:], [128, 4, 512], BF16, [("vln", k) for k in range(4)])
                  dump("uT", uT[:], [128, 4, TB], BF16, ["uT"])
                  dump("widx", widx[:], [128, 4, 8], F32, [("widx", k) for k in range(4)])
              ckpt("p6")
              ckpt("b%dp6" % b)

              for tt_ in range(4):
                  i = b * 4 + tt_
                  tsl = slice(tt_ * 128, (tt_ + 1) * 128)
                  n = (i + 1) * 128
                  if i >= 2:
                      for h in range(8):
                          Sd.ts("pool", Dg[:, h, :], ident_b, widx[:, tt_, h:h + 1], None, ALU.mult, None,
                                ["cbf", ("widx", tt_)], [("Dg", h)])
                      nkb = (n + 511) // 512
                      for kb in range(nkb):
                          k0 = kb * 512
                          kw = min(512, n - k0)
                          pacc, pacck = pacc_t[:, :], [("pacc", 0)]
                          for h in range(8):
                              c, hh = h // 2, h % 2
                              pr = slice(hh * 64, (hh + 1) * 64)
                              pS, pSk = pbank()
                              Sd.mm(pS[:, 0:kw], qiT[pr, c, tsl], kidx2[pr, k0:k0 + kw], True, True,
                                    ["qiT"] + [("kidx2", bb) for bb in range(b + 1)], pSk)
                              rh, rhk = rhslot()
                              Sd.act(rh[:, 0:kw], pS[:, 0:kw], AF.Relu, pSk, [rhk])
                              Sd.mm(pacc[:, 0:kw], Dg[:, h, :], rh[:, 0:kw], h == 0, h == 7, [("Dg", h), rhk], pacck)
                          if k0 + kw == n and kw > 128:
                              Sd.copy("act", isc[:, k0:n - 128], pacc[:, 0:kw - 128], pacck, ["isc"])
                          elif k0 + kw < n:
                              Sd.copy("act", isc[:, k0:k0 + kw], pacc[:, 0:kw], pacck, ["isc"])
                          if k0 + kw == n:
                              Sd.tt("dve", isc[:, n - 128:n], pacc[:, kw - 128:kw], tri_neg, ALU.add, pacck + ["cf32"], ["isc"])
                      ckpt("t%da" % i)
                      lo, hi, mid, cnt, ge, tmp = (stats[:, 10:11], stats[:, 11:12], stats[:, 12:13],
                                                   stats[:, 13:14], stats[:, 14:15], stats[:, 15:16])
                      Sd.add("dve", lambda e, n=n: e.tensor_reduce(stats[:, 11:12], isc[:, 0:n], AX.X, ALU.max), ["isc"], ["tk_hi"])
                      Sd.add("dve", lambda e, n=n: e.tensor_reduce(stats[:, 10:11], isc[:, 0:n - 128], AX.X, ALU.min), ["isc"], ["tk_lo"])
                      Sd.ts("dve", hi, hi, 1.0, None, ALU.add, None, ["tk_hi"], ["tk_hi"])
                      for it in range(22):
                          Sd.ts("dve", mid, lo, hi, 0.5, ALU.add, ALU.mult, ["tk_lo", "tk_hi"], ["tk_mid"])
                          Sd.ts("dve", maskb[:, 0:n], isc[:, 0:n], mid, None, ALU.is_ge, ALU.add, ["isc", "tk_mid"],
                                ["maskb", "tk_cnt"], accum_out=cnt)
                          Sd.ts("dve", ge, cnt, float(TOPK) - 0.5, None, ALU.is_ge, None, ["tk_cnt"], ["tk_ge"])
                          Sd.tt("dve", tmp, mid, lo, ALU.subtract, ["tk_mid", "tk_lo"], ["tk_tmp"])
                          Sd.stt("dve", lo, tmp, ge, lo, ALU.mult, ALU.add, ["tk_tmp", "tk_ge", "tk_lo"], ["tk_lo"])
                          Sd.tt("dve", tmp, hi, mid, ALU.subtract, ["tk_hi", "tk_mid"], ["tk_tmp"])
                          Sd.stt("dve", hi, tmp, ge, mid, ALU.mult, ALU.add, ["tk_tmp", "tk_ge", "tk_mid"], ["tk_hi"])
                      Sd.ts("dve", maskb[:, 0:n], isc[:, 0:n], lo, None, ALU.is_ge, None, ["isc", "tk_lo"], ["maskb"])
                      if i == 2 and l == 0:
                          dump("isc", isc[:, 0:n], [128, n], F32, ["isc"])
                          dump("maskb", maskb[:, 0:n], [128, n], BF16, ["maskb"])
                      ckpt("t%db" % i)
                      for j0 in range(0, i + 1, 4):
                          jn = min(4, i + 1 - j0)
                          if stop == "t6c1" and i == 6 and j0 == 4:
                              raise _Stop()
                          ph, phk = pbfhalf()
                          for jj in range(jn):
                              j = j0 + jj
                              Sd.tr(ph[:, jj * 128:(jj + 1) * 128], maskb[:, j * 128:(j + 1) * 128], ident_b,
                                    ["maskb", "cbf"], phk)
                          Sd.copy("dve", maskT[:, j0:j0 + jn, :], ph[:, 0:jn * 128].rearrange("p (j t) -> p j t", j=jn),
                                  phk, ["maskT"])
                  ckpt("t%dc" % i)
                  for half in range(2):
                      Sd.mm(pO[:, half * 512:half * 512 + 260], Zt[:, 0:128], Zt[:, 0:260], True, False,
                            ["Zt"], [("pO", half)])
                  for j in range(i + 1):
                      ssl = slice(j * 128, (j + 1) * 128)
                      psc, psck = pbank(2)
                      for h in range(8):
                          c, hh = h // 2, h % 2
                          pr = slice(hh * 64, (hh + 1) * 64)
                          pos_ = hh * 4 + c
                          Sd.mm(psc[:, pos_ * 128:(pos_ + 1) * 128], kT_all[pr, c, ssl], qT[pr, c, tsl], True, True,
                                [("kT", j // 4), "qT"], psck)
                      pe_, pt_, pek, ptk = ppslot()
                      Sd.act(pe_[:].rearrange("p h t -> p (h t)"), psc, AF.Exp, psck, [pek], scale=0.125)
                      if i < 2:
                          if j == i:
                              mk = triT.unsqueeze(1).to_broadcast([128, 8, 128])
                              Sd.tt("dve", pt_[:], pe_[:], mk, ALU.mult, [pek, "cbf"], [ptk])
                              src, srck = pt_, ptk
                          else:
                              src, srck = pe_, pek
                      else:
                          mk = maskT[:, j, :].unsqueeze(1).to_broadcast([128, 8, 128])
                          Sd.tt("dve", pt_[:], pe_[:], mk, ALU.mult, [pek, "maskT"], [ptk])
                          src, srck = pt_, ptk
                      for h in range(8):
                          half = h // 4
                          o = pO[:, half * 512 + (h % 4) * 65: half * 512 + (h % 4) * 65 + 65]
                          Sd.mm(o, src[:, (h % 2) * 4 + h // 2, :], Vt[:, j, h * 65:(h + 1) * 65], False, j == i,
                                [srck, ("Vt", j)], [("pO", half)])
                  ckpt("t%dd" % i)
                  for half in range(2):
                      ov = pO[:, half * 512:half * 512 + 260].rearrange("p (h d) -> p h d", h=4)
                      Sd.add("dve", lambda e, ov=ov, half=half: e.reciprocal(rden[:, half * 4:half * 4 + 4].unsqueeze(2), ov[:, :, 64:65]),
                             [("pO", half)], [("rden", half)])
                      Sd.tt("dve", ya[:, half * 256:(half + 1) * 256].rearrange("p (h d) -> p h d", h=4), ov[:, :, 0:64],
                            rden[:, half * 4:half * 4 + 4].unsqueeze(2).to_broadcast([128, 4, 64]), ALU.mult,
                            [("pO", half), ("rden", half)], ["ya"])
                  ph, phk = pbfhalf()
                  for c in range(4):
                      Sd.tr(ph[:, c * 128:(c + 1) * 128], ya[:, c * 128:(c + 1) * 128], ident_b, ["ya", "cbf"], phk)
                  Sd.copy("act", yaT[:, :, tsl], ph.rearrange("p (c t) -> p c t", c=4), phk, ["yaT"])
                  for c in range(4):
                      pt, pk = pbank()
                      for hh in range(2):
                          g = c * 2 + hh
                          o = pt[hh * 64:(hh + 1) * 64, 0:128]
                          Sd.mm(o, vln[:, tt_, g * 64:(g + 1) * 64], wsTm[:, g * 128:(g + 1) * 128], True, False,
                                [("vln", tt_), "wsTm"], pk)
                          Sd.mm(o, one128[:, :], bs128[:, g * 128:(g + 1) * 128], False, True,
                                ["one128", "bs128"], pk)
                      sp_, spk = rhslot()
                      Sd.copy("act", sp_[:, 0:128], pt[:, 0:128], pk, [spk])
                      Sd.tt("dve", ybT[:, c, tsl], sp_[:, 0:128], uT[:, c, tsl], ALU.mult, [spk, "uT"], ["ybT"])
              if l == 0 and b == 0:
                  dump("yaT", yaT[:], [128, 4, TB], BF16, ["yaT"])
                  dump("ybT", ybT[:], [128, 4, TB], BF16, ["ybT"])
              ckpt("p7")

              wpav = wpa_d[l].rearrange("(kc p) f -> p kc f", p=128)
              wpbv = wpb_d[l].rearrange("(kc p) f -> p kc f", p=128)
              for gh in range(2):
                  wga, wgak = load_w(l, "ga%d" % gh)
                  wpa, wpak = load_w(l, "pa%d" % gh)
                  for c in range(4):
                      fo = gh * 4 + c
                      pt, pk = proj_fm(wga, wgak, c * 128, 128, 8, hrhs, ["hT"])
                      sa, sak = fscr()
                      Sd.act(sa[:], pt, AF.Sigmoid, pk, [sak])
                      pa, pak = proj_fm(wpa, wpak, c * 128, 128, 4, lambda kc: yaT[:, kc, :], ["yaT"])
                      Sd.tt("dve", mergedT[:, fo, :], pa, sa[:], ALU.mult, pak + [sak], [("mergedT", fo)])
                  wgb, wgbk = load_w(l, "gb%d" % gh)
                  wpb, wpbk = load_w(l, "pb%d" % gh)
                  for c in range(4):
                      fo = gh * 4 + c
                      pt, pk = proj_fm(wgb, wgbk, c * 128, 128, 8, hrhs, ["hT"])
                      sbt, sbk = fscr()
                      Sd.act(sbt[:], pt, AF.Sigmoid, pk, [sbk])
                      pb_, pbk = proj_fm(wpb, wpbk, c * 128, 128, 4, lambda kc: ybT[:, kc, :], ["ybT"])
                      m2, m2k = fscr()
                      Sd.tt("dve", m2[:], pb_, sbt[:], ALU.mult, pbk + [sbk], [m2k])
                      Sd.tt("pool", mergedT[:, fo, :], m2[:], mergedT[:, fo, :], ALU.add, [m2k, ("mergedT", fo)], [("mergedT", fo)])
              for oh in range(2):
                  wo, wok = load_w(l, "wo%d" % oh)
                  for c in range(4):
                      fo = oh * 4 + c
                      pt, pk = proj_fm(wo, wok, c * 128, 128, 8, lambda kc: mergedT[:, kc, :], [("mergedT", k_) for k_ in range(8)])
                      Sd.tt("dve", xT[:, fo, blk], pt, xT[:, fo, blk], ALU.add, pk + [("xT", b)], [("xT", b)])
              if l == 0 and b == 0:
                  dump("x1", xT[:, :, 0:TB], [128, 8, TB], F32, [("xT", 0)])
              ckpt("p9")

              rmsnorm_block(b, 16 + l * 8, hT, "hT")
              for j in range(6):
                  ncol = 512 if j < 5 else 256
                  wgv, wgk = load_w(l, "wg%d" % j)
                  wuv_, wuk_ = load_w(l, "wu%d" % j)
                  for c in range(ncol // 128):
                      fc = j * 4 + c
                      pg, pgk = proj_fm(wgv, wgk, c * 128, 128, 8, hrhs, ["hT"])
                      pu, puk = proj_fm(wuv_, wuk_, c * 128, 128, 8, hrhs, ["hT"])
                      sg, sgk = fscr()
                      Sd.act(sg[:], pg, AF.Silu, pgk, [sgk])
                      Sd.tt("dve", hid[:, fc, :], pu, sg[:], ALU.mult, puk + [sgk], [("hid", fc)])
              wdv = wd_d[l].rearrange("(fc p) d -> p fc d", p=128)
              for fo in range(8):
                  view, wk = load_w(l, "wd%d" % fo)
                  pt, pk = pbank()
                  for fc in range(NFC):
                      Sd.mm(pt, view[:, fc, :], hid[:, fc, :], fc == 0, fc == NFC - 1, [wk, ("hid", fc)], pk)
                  Sd.tt("dve", xT[:, fo, blk], pt, xT[:, fo, blk], ALU.add, pk + [("xT", b)], [("xT", b)])
              if l == 0 and b == 0:
                  dump("x2", xT[:, :, 0:TB], [128, 8, TB], F32, [("xT", 0)])
              ckpt("p10")
              ckpt("b%d" % b)


    except _Stop:
        pass

    final_keys = []
    onorm = fs
    oT = hidflat[:, 0:8192].bitcast(F32).rearrange("p (k t) -> p k t", k=8)
    ost = [hidflat[:, 8192:10240].bitcast(F32)]
    for b in range(NB):
        hidkeys = [("hid", fc) for fc in range(NFC)]
        blk = slice(b * TB, (b + 1) * TB)
        pt, pk = pbank()
        for kc in range(8):
            sq, sqk = rhslot()
            Sd.act(sq[:], xT[:, kc, blk], AF.Square, [("xT", b)], [sqk])
            Sd.mm(pt, ones_b, sq[:], kc == 0, kc == 7, [sqk, "cbf"], pk)
        ms, msk = fscr()
        Sd.ts("dve", ms[:], pt, 1.0 / D, 1e-6, ALU.mult, ALU.add, pk, [msk])
        rB, rk = fscr()
        Sd.tt("pool", rB[:], ms[:], small[:, 0:1].to_broadcast([128, TB]), ALU.pow, [msk, "small"], [rk])
        for kc in range(8):
            Sd.stt("dve", oT[:, kc, :], xT[:, kc, blk], gains[:, 32 + kc:33 + kc], rB[:],
                   ALU.mult, ALU.mult, [("xT", b), rk, "gains"] , hidkeys)
        for tt_ in range(4):
            i = b * 4 + tt_
            st = ost[0]
            for half in range(2):
                pt, pk = pbank()
                for c in range(4):
                    kc = half * 4 + c
                    Sd.tr(pt[:, c * 128:(c + 1) * 128], oT[:, kc, tt_ * 128:(tt_ + 1) * 128], ident_f, hidkeys + ["cf32"], pk)
                Sd.copy("dve" if half == 0 else "act", st[:, half * 512:(half + 1) * 512], pt, pk, ["ost"])
            Sd.dma("sp", out_d[i * 128:(i + 1) * 128, :], st, ["ost"], [("out", i)])
            final_keys.append(("out", i))
    for name, (tn, key) in dbg_outs.items():
        final_keys.append(key)

    Sd.emit(stack, final_keys)
    stack.close()
    return nc, Sd, dbg_outs


def _consts():
    ident = np.eye(128, dtype=np.float32)
    t = np.arange(128)
    tri_neg = np.where(t[None, :] <= t[:, None], 0.0, NEG).astype(np.float32)
    ones = np.ones((128, 128), np.float32)
    cf32 = np.concatenate([ident, tri_neg, ones], axis=1)
    triT = (t[:, None] <= t[None, :]).astype(np.float32)
    R = np.zeros((128, 128), np.float32)
    for base in (0, 64):
        for d in range(8):
            R[base + d + 8, base + d] = -1.0
            R[base + d, base + d + 8] = 1.0
    E = np.zeros((128, 128), np.float32)
    for d in range(16):
        E[d, d] = 1.0
    cbf = np.concatenate([ident, triT, R, E, ones], axis=1).astype(ml_dtypes.bfloat16)
    return cf32, cbf


def prep_shared(inputs):
    f = lambda k: np.asarray(inputs[k], dtype=np.float32)
    w_in = f("w_in")
    o_q, o_ckv, o_kr, o_qi, o_ki, o_wi, o_uv, o_g = 0, 512, 640, 656, 1168, 1232, 1240, 2264
    cols = np.concatenate([
        np.arange(o_q, o_q + 512),
        np.arange(o_qi, o_qi + 512),
        np.arange(o_ki, o_ki + 64), np.arange(o_ki, o_ki + 64),
        np.arange(o_ckv, o_ckv + 128),
        np.arange(o_kr, o_kr + 16),
        np.arange(o_uv, o_uv + 512),
        np.arange(o_g, o_g + 2048),
        np.arange(o_uv + 512, o_uv + 1024),
        np.arange(o_wi, o_wi + 8),
    ])
    assert cols.shape[0] == WIN_COLS
    w_in_r = np.ascontiguousarray(w_in[:, :, cols])
    w_uk = f("w_uk")
    w_ukT = np.zeros((L, 128, 8, 64), np.float32)
    w_ukT[:, :, :, 16:64] = np.transpose(w_uk, (0, 3, 1, 2))
    w_uvr = np.ascontiguousarray(np.transpose(f("w_uv"), (0, 2, 1, 3))).reshape(L, 128, 512)
    w_sT = np.ascontiguousarray(np.transpose(f("w_s"), (0, 3, 1, 2))).reshape(L, 128, 1024)
    b_sr = f("b_s").reshape(L, 1, 1024)
    gains = np.zeros((128, 48), np.float32)
    nm, nf, fn, kvn = f("norm_mix"), f("norm_ffn"), f("final_norm"), f("kv_norm")
    for l in range(L):
        gains[:, l * 8:(l + 1) * 8] = nm[l].reshape(8, 128).T
        gains[:, 16 + l * 8:16 + (l + 1) * 8] = nf[l].reshape(8, 128).T
        gains[:, 40 + l] = kvn[l]
    gains[:, 32:40] = fn.reshape(8, 128).T
    inv = (THETA ** (-np.arange(0, 16, 2, dtype=np.float32) / 16)).astype(np.float32)
    for base in (0, 64):
        for d in range(16):
            gains[base + d, 42] = inv[d % 8]
    cf32, cbf = _consts()
    return {
        "w_in_r": w_in_r, "w_ukT": w_ukT.reshape(L, 128, 512), "w_uvr": w_uvr, "w_sT": w_sT, "b_sr": b_sr,
        "ln_g": f("ln_v_g").reshape(L, 1, 512), "ln_b": f("ln_v_b").reshape(L, 1, 512),
        "w_pa": f("w_proj_a"), "w_pb": f("w_proj_b"), "w_out": f("w_out"),
        "w_gate": f("w_gate"), "w_up": f("w_up"), "w_down": f("w_down"),
        "gains": gains, "cf32": cf32, "cbf": cbf,
    }


_CACHE = {}


def kernel(**inputs):
    shared = prep_shared(inputs)
    x = np.asarray(inputs["x"], dtype=np.float32)
    pos = np.asarray(inputs["positions"]).astype(np.int32)
    if "nc" not in _CACHE:
        _CACHE["nc"] = build_nc()[0]
    nc = _CACHE["nc"]
    in_maps = []
    for c in range(8):
        m = dict(shared)
        m["x"] = np.ascontiguousarray(x[c])
        m["pos"] = np.ascontiguousarray(pos[c].reshape(1, S))
        in_maps.append(m)
    res = run_bass_kernel_spmd(nc, in_maps, core_ids=list(range(8)))
    out = np.stack([np.asarray(r["out"], dtype=np.float32) for r in res.results], axis=0)
    return out
```

```python
import contextlib
import numpy as np
import ml_dtypes
import concourse.bass as bass
import concourse.mybir as mybir
from concourse.bass_utils import run_bass_kernel_spmd

F32 = mybir.dt.float32
BF16 = mybir.dt.bfloat16
I32 = mybir.dt.int32
ALU = mybir.AluOpType
AF = mybir.ActivationFunctionType
AX = mybir.AxisListType

D = 1024
S = 2048
L = 2
NT = 16
TB = 512
NB = S // TB
DFF = 2816
NFC = DFF // 128
TOPK = 256
THETA = 500000.0
NEG = -30000.0

OFF_Q, OFF_QI, OFF_KX, OFF_CKV, OFF_KR = 0, 512, 1024, 1152, 1280
OFF_U, OFF_GA, OFF_GB, OFF_V, OFF_WI = 1296, 1808, 2832, 3856, 4368
WIN_COLS = 4376


class Sched:
    def __init__(self, nc):
        self.nc = nc
        self.ops = []

    def add(self, eng, fn, r=(), w=(), dma=False):
        self.ops.append({"eng": eng, "fn": fn, "r": list(r), "w": list(w), "dma": dma})

    def mm(self, out, lhsT, rhs, start, stop, r, w):
        self.add("pe", lambda e: e.matmul(out, lhsT, rhs, start=start, stop=stop, skip_group_check=True), r, w)

    def tr(self, out, in_, ident, r, w):
        self.add("pe", lambda e: e.transpose(out, in_, ident), r, w)

    def act(self, out, in_, func, r, w, bias=None, scale=None, accum_out=None):
        kw = {}
        if bias is not None:
            kw["bias"] = bias
        if scale is not None:
            kw["scale"] = scale
        if accum_out is not None:
            kw["accum_out"] = accum_out
        self.add("act", lambda e: e.activation(out, in_, func, **kw), r, w)

    def tt(self, eng, out, in0, in1, op, r, w):
        self.add(eng, lambda e: e.tensor_tensor(out, in0, in1, op), r, w)

    def ts(self, eng, out, in0, s1, s2, op0, op1, r, w, accum_out=None):
        if accum_out is not None:
            self.add(eng, lambda e: e.tensor_scalar(out, in0, s1, s2, op0, op1, accum_out=accum_out), r, w)
        elif op1 is None:
            self.add(eng, lambda e: e.tensor_scalar(out, in0, s1, None, op0), r, w)
        else:
            self.add(eng, lambda e: e.tensor_scalar(out, in0, s1, s2, op0, op1), r, w)

    def stt(self, eng, out, in0, scalar, in1, op0, op1, r, w):
        self.add(eng, lambda e: e.scalar_tensor_tensor(out, in0, scalar, in1, op0, op1), r, w)

    def copy(self, eng, out, in_, r, w):
        if eng == "act":
            self.add(eng, lambda e: e.copy(out, in_), r, w)
        else:
            self.add(eng, lambda e: e.tensor_copy(out, in_), r, w)

    def memset(self, eng, ap, val, w):
        self.add(eng, lambda e: e.memset(ap, val), (), w)

    def dma(self, eng, out, in_, r, w):
        self.add(eng, lambda e: e.dma_start(out=out, in_=in_), r, w, dma=True)

    def emit(self, stack, final_keys):
        nc = self.nc
        ops = self.ops
        engs = ["pe", "act", "dve", "pool", "sp"]
        pos_ctr = {e: 0 for e in engs}
        last_w = {}
        readers = {}
        for i, op in enumerate(ops):
            op["pos"] = pos_ctr[op["eng"]]
            pos_ctr[op["eng"]] += 1
            deps = {}

            def add_dep(j, kind):
                if j == i:
                    return
                prev = deps.get(j)
                if prev is None or (prev == "war" and kind != "war"):
                    deps[j] = kind

            for k in op["r"]:
                if k in last_w:
                    add_dep(last_w[k], "raw")
            for k in op["w"]:
                if k in last_w:
                    add_dep(last_w[k], "waw")
                for j in readers.get(k, {}).values():
                    add_dep(j, "war")
            for k in op["r"]:
                readers.setdefault(k, {})[op["eng"]] = i
            for k in op["w"]:
                last_w[k] = i
                readers[k] = {}
            op["deps"] = deps
            op["signal"] = False

        def needs_sync(j, i, kind):
            oj, oi = ops[j], ops[i]
            if oj["dma"]:
                return True
            if oj["eng"] != oi["eng"]:
                return True
            if oi["eng"] == "pe":
                return False
            if oi["eng"] == "pool":
                return True
            if kind == "war":
                return False
            return (oi["pos"] - oj["pos"]) <= 3

        for i, op in enumerate(ops):
            op["sdeps"] = [j for j, kind in op["deps"].items() if needs_sync(j, i, kind)]
            for j in op["sdeps"]:
                ops[j]["signal"] = True
        final_ops = [last_w[k] for k in final_keys]
        for j in final_ops:
            ops[j]["signal"] = True

        esem = {e: stack.enter_context(nc.semaphore("sem_" + e)) for e in engs}
        NS = 12
        dsem = [stack.enter_context(nc.semaphore("dsem%d" % k)) for k in range(NS)]
        ecount = {e: 0 for e in engs}
        dcount = 0
        for op in ops:
            if op["dma"]:
                k = dcount
                dcount += 1
                op["sig"] = (dsem[k % NS], 16 * (k // NS + 1))
                op["dprev"] = (dsem[k % NS], 16 * (k // NS)) if k >= NS else None
            elif op["signal"]:
                ecount[op["eng"]] += 1
                op["sig"] = (esem[op["eng"]], ecount[op["eng"]])
        self.stats = {e: pos_ctr[e] for e in engs}
        self.stats["signals"] = dict(ecount)

        def run_engine(ename, eng):
            waited = {}
            for op in ops:
                if op["eng"] != ename:
                    continue
                waits = [ops[j]["sig"] for j in op["sdeps"]]
                if op["dma"] and op["dprev"] is not None:
                    waits.append(op["dprev"])
                best = {}
                for sem, val in waits:
                    key = id(sem)
                    if key not in best or best[key][1] < val:
                        best[key] = (sem, val)
                for key, (sem, val) in best.items():
                    if waited.get(key, 0) >= val:
                        continue
                    eng.wait_ge(sem, val)
                    waited[key] = val
                if op.get("pad"):
                    if ename == "dve":
                        eng.memset(self.dummy[:, 0:1], 0.0)
                    elif ename == "act":
                        eng.memzero(self.dummy[:, 1:2])
                ins = op["fn"](eng)
                if op["dma"]:
                    ins.then_inc(op["sig"][0], 16)
                elif op["signal"]:
                    ins.then_inc(op["sig"][0], 1)
            if ename == "sp":
                for j in final_ops:
                    sem, val = ops[j]["sig"]
                    eng.wait_ge(sem, val)

        with nc.Block() as block:
            @block.tensor
            def _(e):
                run_engine("pe", e)

            @block.scalar
            def _(e):
                run_engine("act", e)

            @block.vector
            def _(e):
                run_engine("dve", e)

            @block.gpsimd
            def _(e):
                run_engine("pool", e)

            @block.sync
            def _(e):
                run_engine("sp", e)


class _Stop(Exception):
    pass


def build_nc(n_layers=L, dbg=None, stop=None):
    nc = bass.Bass("TRN2", target_bir_lowering=False)
    stack = contextlib.ExitStack()
    Sd = Sched(nc)

    def din(name, shape, dt=F32):
        return nc.dram_tensor(name, list(shape), dt, kind="ExternalInput").ap()

    x_d = din("x", [S, D])
    pos_d = din("pos", [1, S], I32)
    win_d = din("w_in_r", [L, D, WIN_COLS])
    wuk_d = din("w_ukT", [L, 128, 8 * 64])
    wuv_d = din("w_uvr", [L, 128, 8 * 64])
    wsT_d = din("w_sT", [L, 128, 8 * 128])
    bs_d = din("b_sr", [L, 1, 8 * 128])
    lng_d = din("ln_g", [L, 1, 512])
    lnb_d = din("ln_b", [L, 1, 512])
    wpa_d = din("w_pa", [L, 512, D])
    wpb_d = din("w_pb", [L, 512, D])
    wout_d = din("w_out", [L, D, D])
    wg_d = din("w_gate", [L, D, DFF])
    wu_d = din("w_up", [L, D, DFF])
    wd_d = din("w_down", [L, DFF, D])
    gains_d = din("gains", [128, 80])
    cf32_d = din("cf32", [128, 3 * 128])
    cbf_d = din("cbf", [128, 5 * 128], BF16)
    out_d = nc.dram_tensor("out", [S, D], F32, kind="ExternalOutput").ap()

    def sb(name, shape, dt):
        return stack.enter_context(nc.sbuf_tensor("s_" + name, list(shape), dt))

    def ps(name, shape, dt):
        return stack.enter_context(nc.psum_tensor(name, list(shape), dt))

    xT = sb("xT", [128, 8, S], F32)
    hT = sb("hT", [128, 8, TB], BF16)
    kT_all = sb("kT_all", [128, 4, S], BF16)
    Vt = sb("Vt", [128, NT, 8 * 65], BF16)
    kidx2 = sb("kidx2", [128, S], BF16)
    ckvn = sb("ckvn", [128, TB], BF16)
    kropeT = sb("kropeT", [128, TB], BF16)
    Zt = sb("Zt", [128, 512], BF16)
    bs128 = sb("bs128", [128, 8 * 128], BF16)
    one128 = sb("one128", [128, 64], BF16)
    rden = sb("rden", [128, 8], F32)
    qv = sb("qv", [128, 8, TB], BF16)
    qT = qv[:, 0:4, :]
    vln = qv[:, 4:8, :]
    mergedT = qv
    qiT = sb("qiT", [128, 4, TB], BF16)
    yaT = qiT
    uT = sb("uT", [128, 4, TB], BF16)
    ybT = uT
    widx = sb("widx", [128, 4, 8], F32)
    arena = sb("arena", [128, NFC * TB], BF16)
    hid = arena[:].rearrange("p (a b) -> p a b", a=NFC)
    isc = arena[:, 0:4096].bitcast(F32)
    maskb = arena[:, 4096:6144]
    maskT = arena[:, 6144:8192].rearrange("p (j t) -> p j t", j=NT)
    Pexp = [arena[:, 8192:9216].rearrange("p (h t) -> p h t", h=8)]
    PT = [arena[:, 9216:10240].rearrange("p (h t) -> p h t", h=8)]
    zb = arena[:, 10240:10752]
    ya = arena[:, 10752:11264]
    Rh = [sb("Rh%d" % k, [128, 512], BF16) for k in range(2)]
    Dg = sb("Dg", [128, 8, 128], BF16)
    NF = 6
    fs = [sb("fs%d" % k, [128, 512], F32) for k in range(NF)]
    NW = 3
    wt = [sb("wt%d" % k, [128, 8 * 520], BF16) for k in range(NW)]
    Ctab = sb("Ctab", [128, S], BF16)
    Stab = sb("Stab", [128, S], BF16)
    wsTm = sb("wsTm", [128, 8 * 128], BF16)
    wukT = sb("wukT", [128, 8 * 64], BF16)
    wuvr = sb("wuvr", [128, 8 * 64], BF16)
    gains = sb("gains", [128, 80], F32)
    cf32 = sb("cf32", [128, 3 * 128], F32)
    cbf = sb("cbf", [128, 5 * 128], BF16)
    small = sb("small", [128, 64], F32)
    Sd.dummy = small[:, 32:34]
    stats = sb("stats", [128, 16], F32)

    ident_f = cf32[:, 0:128]
    tri_neg = cf32[:, 128:256]
    ones_f = cf32[:, 256:384]
    ident_b = cbf[:, 0:128]
    triT = cbf[:, 128:256]
    Rrot = cbf[:, 256:384]
    E16 = cbf[:, 384:448]
    ones_b = cbf[:, 512:640]

    pmain = ps("pmain", [128, 4 * 512], F32)
    pacc_t = ps("pacc_t", [128, 512], F32)
    pbf = ps("pbf", [128, 1024], BF16)
    pO = ps("pO", [128, 2 * 512], F32)

    state = {"pb": 0, "fs": 0, "wt": 0, "rh": 0, "pbf": 0, "pp": 0}

    def pbank(n=1):
        b = state["pb"]
        if b + n > 4:
            b = 0
        state["pb"] = (b + n) % 4
        return pmain[:, b * 512:(b + n) * 512], [("ps", b + k) for k in range(n)]

    def pbfhalf():
        h = state["pbf"]
        state["pbf"] = 1 - h
        return pbf[:, h * 512:(h + 1) * 512], [("pbf", 0)]

    def fscr():
        k = state["fs"]
        state["fs"] = (k + 1) % NF
        return fs[k], ("fs", k)

    def wslot():
        k = state["wt"]
        state["wt"] = (k + 1) % NW
        return wt[k], ("wt", k)

    def rhslot():
        k = state["rh"]
        state["rh"] = (k + 1) % 2
        return Rh[k], ("rh", k)

    def ppslot():
        k = 0
        return Pexp[k], PT[k], ("pexp", k), ("pt", k)

    dbg_outs = {}

    def dump(name, ap, shape, dt, keys):
        if dbg is None or name not in dbg:
            return
        t = nc.dram_tensor("dbg_" + name, list(shape), dt, kind="ExternalOutput").ap()
        Sd.dma("sp", t, ap, keys, [("dbg", name)])
        dbg_outs[name] = ("dbg_" + name, ("dbg", name))

    Sd.dma("sp", gains[:], gains_d, [], ["gains"])
    Sd.dma("sp", cf32[:], cf32_d, [], ["cf32"])
    Sd.dma("sp", cbf[:], cbf_d, [], ["cbf"])
    Sd.memset("dve", Zt[:], 0.0, ["Zt"])
    Sd.memset("dve", kropeT[:], 0.0, ["kropeT"])
    Sd.memset("dve", bs128[:], 0.0, ["bs128"])
    Sd.memset("dve", one128[:], 0.0, ["one128"])
    Sd.memset("dve", one128[0:1, :], 1.0, ["one128"])

    posi = arena[:, 4096:8192].bitcast(I32)
    Sd.dma("sp", posi[:], pos_d.partition_broadcast(128), [], ["posi"])
    invf = gains[:, 42:43]
    PI = float(np.pi)
    MAGIC = 12582912.0
    HI = 6.28125
    LO = float(2 * np.pi - 6.28125)

    def sin_of(dst, ang, angk, dkey):
        t, tk = fs[2], ("fs", 2)
        r, rk_ = fs[3], ("fs", 3)
        a, ak = fs[4], ("fs", 4)
        c, ck = fs[5], ("fs", 5)
        Sd.ts("dve", t[:], ang[:], 1.0 / (2 * PI), MAGIC, ALU.mult, ALU.add, [angk], [tk])
        Sd.ts("dve", r[:], t[:], MAGIC, None, ALU.subtract, None, [tk], [rk_])
        Sd.stt("dve", a[:], r[:], -HI, ang[:], ALU.mult, ALU.add, [rk_, angk], [ak])
        Sd.stt("dve", c[:], r[:], -LO, a[:], ALU.mult, ALU.add, [rk_, ak], [ck])
        Sd.ts("dve", a[:], c[:], PI, -PI, ALU.min, ALU.max, [ck], [ak])
        Sd.act(dst, a[:], AF.Sin, [ak], [dkey])

    for half in range(4):
        sl = slice(half * 512, (half + 1) * 512)
        a0, k0 = fs[0], ("fs", 0)
        a1, k1 = fs[1], ("fs", 1)
        Sd.copy("dve", a0[:], posi[:, sl], ["posi"], [k0])
        Sd.ts("dve", a1[:], a0[:], invf, None, ALU.mult, None, [k0, "gains"], [k1])
        sin_of(Stab[:, sl], a1, k1, ("Stab", half))
        Sd.ts("dve", a0[:], a1[:], 0.5 * PI, None, ALU.add, None, [k1], [k0])
        sin_of(Ctab[:, sl], a0, k0, ("Ctab", half))

    hidflat = arena
    xin = [hidflat[:, 0:2048].bitcast(F32), hidflat[:, 2048:4096].bitcast(F32)]
    for i in range(NT):
        st = xin[i % 2]
        sk = ("xin", i % 2)
        Sd.dma("sp", st, x_d[i * 128:(i + 1) * 128, :], [], [sk])
        for half in range(2):
            pt, pk = pbank()
            for c in range(4):
                kc = half * 4 + c
                Sd.tr(pt[:, c * 128:(c + 1) * 128], st[:, kc * 128:(kc + 1) * 128], ident_f, [sk, "cf32"], pk)
            dst = xT[:, half * 4:(half + 1) * 4, i * 128:(i + 1) * 128]
            src = pt.rearrange("p (c t) -> p c t", c=4)
            Sd.copy("dve" if half == 0 else "act", dst, src, pk, [("xT", i // 4)])

    def rmsnorm_block(b, gcol0, dst, dst_key, extra_scale_keys=()):
        blk = slice(b * TB, (b + 1) * TB)
        pt, pk = pbank()
        for kc in range(8):
            sq, sqk = rhslot()
            Sd.act(sq[:], xT[:, kc, blk], AF.Square, [("xT", b)], [sqk])
            Sd.mm(pt, ones_b, sq[:], kc == 0, kc == 7, [sqk, "cbf"], pk)
        ms, msk = fscr()
        Sd.ts("dve", ms[:], pt, 1.0 / D, 1e-6, ALU.mult, ALU.add, pk, [msk])
        rB, rk = fscr()
        Sd.tt("pool", rB[:], ms[:], small[:, 0:1].to_broadcast([128, TB]), ALU.pow, [msk, "small"], [rk])
        for kc in range(8):
            Sd.stt("dve", dst[:, kc, :], xT[:, kc, blk], gains[:, gcol0 + kc:gcol0 + kc + 1], rB[:],
                   ALU.mult, ALU.mult, [("xT", b), rk, "gains"], [dst_key])

    Sd.memset("dve", small[:, 0:1], -0.5, ["small"])

    scratch = {}
    stage = [(kT_all[:].rearrange("p a b -> p (a b)").bitcast(F32), "stage0"),
             (Vt[:].rearrange("p a b -> p (a b)").bitcast(F32), "stage1")]
    pstate = {"i": 0}
    cast_engs = ["dve", "act", "pool"]

    def prologue_chunk(l, name, src_ap):
        kcn, ncol = src_ap.shape[1], src_ap.shape[2]
        n = kcn * ncol
        sc = nc.dram_tensor("sc_%d_%s" % (l, name), [128, n], BF16).ap()
        i = pstate["i"]
        pstate["i"] += 1
        st, stk = stage[i % 2]
        Sd.dma("sp", st[:, 0:n].rearrange("p (k n) -> p k n", k=kcn), src_ap, [], [stk])
        w, wk = wslot()
        Sd.copy(cast_engs[i % 3], w[:, 0:n], st[:, 0:n], [stk], [wk])
        Sd.dma("sp", sc, w[:, 0:n], [wk], [("sc", l, name)])
        scratch[(l, name)] = (sc, kcn, ncol)

    def load_w(l, name):
        sc, kcn, ncol = scratch[(l, name)]
        w, wk = wslot()
        n = kcn * ncol
        Sd.dma("sp", w[:, 0:n], sc, [("sc", l, name)], [wk])
        return w[:, 0:n].rearrange("p (k n) -> p k n", k=kcn), wk

    for l in range(n_layers):
        winv = win_d[l].rearrange("(kc p) f -> p kc f", p=128)
        prologue_chunk(l, "kx", winv[:, :, OFF_KX:OFF_KX + 272])
        prologue_chunk(l, "q", winv[:, :, OFF_Q:OFF_Q + 512])
        prologue_chunk(l, "qi", winv[:, :, OFF_QI:OFF_QI + 512])
        prologue_chunk(l, "v", winv[:, :, OFF_V:OFF_V + 512])
        prologue_chunk(l, "wi", winv[:, :, OFF_WI:OFF_WI + 8])
        prologue_chunk(l, "u", winv[:, :, OFF_U:OFF_U + 512])
        wpav = wpa_d[l].rearrange("(kc p) f -> p kc f", p=128)
        wpbv = wpb_d[l].rearrange("(kc p) f -> p kc f", p=128)
        wov = wout_d[l].rearrange("(kc p) f -> p kc f", p=128)
        for gh in range(2):
            prologue_chunk(l, "ga%d" % gh, winv[:, :, OFF_GA + gh * 512:OFF_GA + (gh + 1) * 512])
            prologue_chunk(l, "gb%d" % gh, winv[:, :, OFF_GB + gh * 512:OFF_GB + (gh + 1) * 512])
            prologue_chunk(l, "pa%d" % gh, wpav[:, :, gh * 512:(gh + 1) * 512])
            prologue_chunk(l, "pb%d" % gh, wpbv[:, :, gh * 512:(gh + 1) * 512])
            prologue_chunk(l, "wo%d" % gh, wov[:, :, gh * 512:(gh + 1) * 512])
        wgv_ = wg_d[l].rearrange("(kc p) f -> p kc f", p=128)
        wuv2_ = wu_d[l].rearrange("(kc p) f -> p kc f", p=128)
        for j in range(6):
            ncol = 512 if j < 5 else 256
            prologue_chunk(l, "wg%d" % j, wgv_[:, :, j * 512:j * 512 + ncol])
            prologue_chunk(l, "wu%d" % j, wuv2_[:, :, j * 512:j * 512 + ncol])
        wdv = wd_d[l].rearrange("(fc p) d -> p fc d", p=128)
        for fo in range(8):
            prologue_chunk(l, "wd%d" % fo, wdv[:, :, fo * 128:(fo + 1) * 128])
    Sd.memset("pool", Vt[:], 1.0, ["Vt_init", "stage1"])
    if stop == "p0":
        n_layers = 0

    def ckpt(name):
        if stop == name:
            raise _Stop()

    try:
      for l in range(n_layers):
          winv = win_d[l].rearrange("(kc p) f -> p kc f", p=128)
          t0, tk0 = fscr()
          Sd.dma("sp", t0[:], wuk_d[l], [], [tk0])
          Sd.copy("dve", wukT[:], t0[:], [tk0], ["wukT"])
          t1, tk1 = fscr()
          Sd.dma("sp", t1[:], wuv_d[l], [], [tk1])
          Sd.copy("dve", wuvr[:], t1[:], [tk1], ["wuvr"])
          for hf in range(2):
              t2, tk2 = fscr()
              Sd.dma("sp", t2[0:1, :], bs_d[l][:, hf * 512:(hf + 1) * 512], [], [tk2])
              Sd.copy("dve", bs128[0:1, hf * 512:(hf + 1) * 512], t2[0:1, :], [tk2], ["bs128"])
              t3, tk3 = fscr()
              Sd.dma("sp", t3[:], wsT_d[l][:, hf * 512:(hf + 1) * 512], [], [tk3])
              Sd.tt("dve", wsTm[:, hf * 512:(hf + 1) * 512].rearrange("p (g t) -> p g t", g=4),
                    t3[:].rearrange("p (g t) -> p g t", g=4),
                    triT.unsqueeze(1).to_broadcast([128, 4, 128]), ALU.mult, [tk3, "cbf"], ["wsTm"])

          for b in range(NB):
              blk = slice(b * TB, (b + 1) * TB)
              rmsnorm_block(b, l * 8, hT, "hT")
              if l == 0 and b == 0:
                  dump("hT", hT[:], [128, 8, TB], BF16, ["hT"])
              ckpt("p1")

              def proj_fm(wview, wk, c0, M, kcn, rhs_of_kc, rkeys):
                  pt, pk = pbank()
                  for kc in range(kcn):
                      Sd.mm(pt[0:M, :], wview[:, kc, c0:c0 + M], rhs_of_kc(kc), kc == 0, kc == kcn - 1,
                            [wk] + rkeys, pk)
                  return pt, pk

              def rope_evac(pt, pk, M, dst, dkey):
                  Sd.copy("act", zb[0:M, :], pt[0:M, :], pk, ["zb"])
                  zs, zsk = rhslot()
                  Sd.tt("dve", zs[0:M, :], zb[0:M, :], Stab[0:M, blk], ALU.mult, ["zb", ("Stab", b)], [zsk])
                  p2, pk2 = pbank()
                  Sd.mm(p2[0:M, :], Rrot[0:M, 0:M], zs[0:M, :], True, True, [zsk, "cbf"], pk2)
                  zc, zck = fscr()
                  Sd.tt("dve", zc[0:M, :], zb[0:M, :], Ctab[0:M, blk], ALU.mult, ["zb", ("Ctab", b)], [zck])
                  Sd.tt("dve", dst, p2[0:M, :], zc[0:M, :], ALU.add, pk2 + [zck], [dkey])

              hrhs = lambda kc: hT[:, kc, :]
              wv, wk = load_w(l, "kx")
              ckpt("p2w")
              pt, pk = proj_fm(wv, wk, 0, 128, 8, hrhs, ["hT"])
              if stop == "p2x":
                  Sd.copy("act", kidx2[:, blk], pt, pk, [("kidx2", b)])
                  dump("kidx2", kidx2[:, 0:TB], [128, TB], BF16, [("kidx2", 0)])
              ckpt("p2x")
              rope_evac(pt, pk, 128, kidx2[:, blk], ("kidx2", b))
              ckpt("p2a")
              pt, pk = proj_fm(wv, wk, 256, 16, 8, hrhs, ["hT"])
              rope_evac(pt, pk, 16, kropeT[0:16, :], "kropeT")
              ckpt("p2b")
              pt, pk = proj_fm(wv, wk, 128, 128, 8, hrhs, ["hT"])
              sq, sqk = rhslot()
              Sd.act(sq[:], pt, AF.Square, pk, [sqk])
              p2, pk2 = pbank()
              Sd.mm(p2, ones_b, sq[:], True, True, [sqk, "cbf"], pk2)
              ms, msk = fscr()
              Sd.ts("dve", ms[:], p2, 1.0 / 128, 1e-6, ALU.mult, ALU.add, pk2, [msk])
              rB, rk = fscr()
              Sd.tt("pool", rB[:], ms[:], small[:, 0:1].to_broadcast([128, TB]), ALU.pow, [msk, "small"], [rk])
              Sd.stt("dve", ckvn[:], pt, gains[:, 40 + l:41 + l], rB[:], ALU.mult, ALU.mult, pk + [rk, "gains"], ["ckvn"])
              ckpt("p2c")
              for pr in range(4):
                  pt, pk = pbank()
                  for hh in range(2):
                      h = pr * 2 + hh
                      o = pt[hh * 64:(hh + 1) * 64, :]
                      Sd.mm(o, wukT[:, h * 64:(h + 1) * 64], ckvn[:], True, False, ["wukT", "ckvn"], pk)
                      Sd.mm(o, E16, kropeT[:, :], False, True, ["cbf", "kropeT"], pk)
                  Sd.copy("act", kT_all[:, pr, blk], pt, pk, [("kT", b), "stage0"])
              ckpt("p2d")
              for tt_ in range(4):
                  i = b * 4 + tt_
                  pt, pk = pbank()
                  Sd.mm(pt, ckvn[:, tt_ * 128:(tt_ + 1) * 128], wuvr[:], True, True, ["ckvn", "wuvr"], pk)
                  dst = Vt[:, i, :].rearrange("p (h d) -> p h d", h=8)[:, :, 0:64]
                  Sd.copy("act", dst, pt.rearrange("p (h d) -> p h d", h=8), pk + ["Vt_init"], [("Vt", i)])
              if l == 0 and b == 0:
                  dump("kidx2", kidx2[:, 0:TB], [128, TB], BF16, [("kidx2", 0)])
                  dump("kT", kT_all[:, :, 0:TB], [128, 4, TB], BF16, [("kT", 0)])
                  dump("Vt", Vt[:, 0:4, :], [128, 4, 520], BF16, [("Vt", k) for k in range(4)])
              ckpt("p2")

              wv, wk = load_w(l, "q")
              for c in range(4):
                  pt, pk = proj_fm(wv, wk, c * 128, 128, 8, hrhs, ["hT"])
                  rope_evac(pt, pk, 128, qT[:, c, :], "qT")
              wv, wk = load_w(l, "qi")
              for c in range(4):
                  pt, pk = proj_fm(wv, wk, c * 128, 128, 8, hrhs, ["hT"])
                  rope_evac(pt, pk, 128, qiT[:, c, :], "qiT")
              if l == 0 and b == 0:
                  dump("qT", qT[:], [128, 4, TB], BF16, ["qT"])
              ckpt("p4")

              wv, wk = load_w(l, "v")
              wiv, wik = load_w(l, "wi")
              lngb, lngk = arena[:, 8192:9216].bitcast(F32), ("pexp", 0)
              Sd.dma("sp", lngb, lng_d[l].partition_broadcast(128), [], [lngk, ("hid", 16), ("hid", 17)])
              lnbb, lnbk = arena[:, 9216:10240].bitcast(F32), ("pt", 0)
              Sd.dma("sp", lnbb, lnb_d[l].partition_broadcast(128), [], [lnbk, ("hid", 18), ("hid", 19)])
              for tt_ in range(4):
                  tsl = slice(tt_ * 128, (tt_ + 1) * 128)
                  pt, pk = pbank()
                  for kc in range(8):
                      Sd.mm(pt, hT[:, kc, tsl], wv[:, kc, 0:512], kc == 0, kc == 7, ["hT", wk], pk)
                  gv, gk = fscr()
                  Sd.act(gv[:], pt, AF.Gelu_apprx_tanh, pk, [gk])
                  Sd.add("dve", lambda e, gv=gv: e.bn_stats(stats[:, 0:6], gv[:]), [gk], ["stats6"])
                  Sd.add("dve", lambda e: e.bn_aggr(stats[:, 6:8], stats[:, 0:6]), ["stats6"], ["stats2"])
                  Sd.ts("dve", stats[:, 8:9], stats[:, 7:8], 1e-5, None, ALU.add, None, ["stats2"], ["stats_v"])
                  Sd.tt("pool", stats[:, 9:10], stats[:, 8:9], small[:, 0:1], ALU.pow, ["stats_v", "small"], ["stats_r"])
                  nv, nk = fscr()
                  Sd.ts("dve", nv[:], gv[:], stats[:, 6:7], stats[:, 9:10], ALU.subtract, ALU.mult,
                        [gk, "stats2", "stats_r"], [nk])
                  n2, nk2 = fscr()
                  Sd.tt("pool", n2[:], nv[:], lngb, ALU.mult, [nk, lngk], [nk2])
                  Sd.tt("pool", vln[:, tt_, :], n2[:], lnbb, ALU.add, [nk2, lnbk], [("vln", tt_)])
                  pt, pk = pbank()
                  for kc in range(8):
                      Sd.mm(pt[:, 0:8], hT[:, kc, tsl], wiv[:, kc, 0:8], kc == 0, kc == 7, ["hT", wik], pk)
                  Sd.copy("dve", widx[:, tt_, :], pt[:, 0:8], pk, [("widx", tt_)])
              wv, wk = load_w(l, "u")
              for c in range(4):
                  pt, pk = proj_fm(wv, wk, c * 128, 128, 8, hrhs, ["hT"])
                  Sd.act(uT[:, c, :], pt, AF.Gelu_apprx_tanh, pk, ["uT"])
              if l == 0 and b == 0:
                  dump("vln", vln[:], [128, 4, 512], BF16, [("vln", k) for k in range(4)])
                  dump("uT", uT[:], [128, 4, TB], BF16, ["uT"])
                  dump("widx", widx[:], [128, 4, 8], F32, [("widx", k) for k in range(4)])
              ckpt("p6")
              ckpt("b%dp6" % b)

              for tt_ in range(4):
                  i = b * 4 + tt_
                  tsl = slice(tt_ * 128, (tt_ + 1) * 128)
                  n = (i + 1) * 128
                  if i >= 2:
                      for h in range(8):
                          Sd.ts("pool", Dg[:, h, :], ident_b, widx[:, tt_, h:h + 1], None, ALU.mult, None,
                                ["cbf", ("widx", tt_)], [("Dg", h)])
                      nkb = (n + 511) // 512
                      for kb in range(nkb):
                          k0 = kb * 512
                          kw = min(512, n - k0)
                          pacc, pacck = pacc_t[:, :], [("pacc", 0)]
                          for h in range(8):
                              c, hh = h // 2, h % 2
                              pr = slice(hh * 64, (hh + 1) * 64)
                              pS, pSk = pbank()
                              Sd.mm(pS[:, 0:kw], qiT[pr, c, tsl], kidx2[pr, k0:k0 + kw], True, True,
                                    ["qiT"] + [("kidx2", bb) for bb in range(b + 1)], pSk)
                              rh, rhk = rhslot()
                              Sd.act(rh[:, 0:kw], pS[:, 0:kw], AF.Relu, pSk, [rhk])
                              Sd.mm(pacc[:, 0:kw], Dg[:, h, :], rh[:, 0:kw], h == 0, h == 7, [("Dg", h), rhk], pacck)
                          if k0 + kw == n and kw > 128:
                              Sd.copy("act", isc[:, k0:n - 128], pacc[:, 0:kw - 128], pacck, ["isc"])
                          elif k0 + kw < n:
                              Sd.copy("act", isc[:, k0:k0 + kw], pacc[:, 0:kw], pacck, ["isc"])
                          if k0 + kw == n:
                              Sd.tt("dve", isc[:, n - 128:n], pacc[:, kw - 128:kw], tri_neg, ALU.add, pacck + ["cf32"], ["isc"])
                      ckpt("t%da" % i)
                      K_IT = 20
                      rmin, rmax, Rg, cand, cnt, tq = (stats[:, 10:11], stats[:, 11:12], stats[:, 12:13],
                                                       stats[:, 13:14], stats[:, 14:15], stats[:, 15:16])
                      sk = small[:, 2:2 + K_IT + 1]
                      Sd.add("dve", lambda e, n=n: e.tensor_reduce(stats[:, 11:12], isc[:, 0:n], AX.X, ALU.max), ["isc"], ["tk_hi"])
                      Sd.add("dve", lambda e, n=n: e.tensor_reduce(stats[:, 10:11], isc[:, 0:n - 128], AX.X, ALU.min), ["isc"], ["tk_lo"])
                      Sd.ts("dve", Rg, rmax, 1.0, rmin, ALU.add, ALU.subtract, ["tk_hi", "tk_lo"], ["tk_R"])
                      Sd.ts("dve", sk, gains[:, 48:48 + K_IT + 1], Rg, None, ALU.mult, None, ["tk_R", "gains"], ["tk_sk"])
                      Sd.tt("dve", cand, rmin, sk[:, 0:1], ALU.add, ["tk_lo", "tk_sk"], ["tk_cand"])
                      for it in range(K_IT):
                          Sd.ts("dve", maskb[:, 0:n], isc[:, 0:n], cand, None, ALU.is_ge, ALU.add, ["isc", "tk_cand"],
                                ["maskb", "tk_cnt"], accum_out=cnt)
                          Sd.ts("dve", tq, cnt, float(TOPK) - 0.5, 0.5, ALU.is_ge, ALU.subtract, ["tk_cnt"], ["tk_t"])
                          Sd.stt("dve", cand, tq, sk[:, it:it + 1], cand, ALU.mult, ALU.add, ["tk_t", "tk_sk", "tk_cand"], ["tk_cand"])
                      Sd.tt("dve", Rg, cand, sk[:, K_IT:K_IT + 1], ALU.subtract, ["tk_cand", "tk_sk"], ["tk_R"])
                      Sd.ts("dve", maskb[:, 0:n], isc[:, 0:n], Rg, None, ALU.is_ge, None, ["isc", "tk_R"], ["maskb"])
                      if i == 2 and l == 0:
                          dump("isc", isc[:, 0:n], [128, n], F32, ["isc"])
                          dump("maskb", maskb[:, 0:n], [128, n], BF16, ["maskb"])
                      ckpt("t%db" % i)
                      for j0 in range(0, i + 1, 4):
                          jn = min(4, i + 1 - j0)
                          if stop == "t6c1" and i == 6 and j0 == 4:
                              raise _Stop()
                          ph, phk = pbfhalf()
                          for jj in range(jn):
                              j = j0 + jj
                              Sd.tr(ph[:, jj * 128:(jj + 1) * 128], maskb[:, j * 128:(j + 1) * 128], ident_b,
                                    ["maskb", "cbf"], phk)
                          Sd.copy("dve", maskT[:, j0:j0 + jn, :], ph[:, 0:jn * 128].rearrange("p (j t) -> p j t", j=jn),
                                  phk, ["maskT"])
                  ckpt("t%dc" % i)
                  for half in range(2):
                      Sd.mm(pO[:, half * 512:half * 512 + 260], Zt[:, 0:128], Zt[:, 0:260], True, False,
                            ["Zt"], [("pO", half)])
                  for j in range(i + 1):
                      ssl = slice(j * 128, (j + 1) * 128)
                      psc, psck = pbank(2)
                      for h in range(8):
                          c, hh = h // 2, h % 2
                          pr = slice(hh * 64, (hh + 1) * 64)
                          pos_ = hh * 4 + c
                          Sd.mm(psc[:, pos_ * 128:(pos_ + 1) * 128], kT_all[pr, c, ssl], qT[pr, c, tsl], True, True,
                                [("kT", j // 4), "qT"], psck)
                      pe_, pt_, pek, ptk = ppslot()
                      Sd.act(pe_[:].rearrange("p h t -> p (h t)"), psc, AF.Exp, psck, [pek], scale=0.125)
                      if i < 2:
                          if j == i:
                              mk = triT.unsqueeze(1).to_broadcast([128, 8, 128])
                              Sd.tt("dve", pt_[:], pe_[:], mk, ALU.mult, [pek, "cbf"], [ptk])
                              src, srck = pt_, ptk
                          else:
                              src, srck = pe_, pek
                      else:
                          mk = maskT[:, j, :].unsqueeze(1).to_broadcast([128, 8, 128])
                          Sd.tt("dve", pt_[:], pe_[:], mk, ALU.mult, [pek, "maskT"], [ptk])
                          src, srck = pt_, ptk
                      for h in range(8):
                          half = h // 4
                          o = pO[:, half * 512 + (h % 4) * 65: half * 512 + (h % 4) * 65 + 65]
                          Sd.mm(o, src[:, (h % 2) * 4 + h // 2, :], Vt[:, j, h * 65:(h + 1) * 65], False, j == i,
                                [srck, ("Vt", j)], [("pO", half)])
                  ckpt("t%dd" % i)
                  for half in range(2):
                      ov = pO[:, half * 512:half * 512 + 260].rearrange("p (h d) -> p h d", h=4)
                      Sd.add("dve", lambda e, ov=ov, half=half: e.reciprocal(rden[:, half * 4:half * 4 + 4].unsqueeze(2), ov[:, :, 64:65]),
                             [("pO", half)], [("rden", half)])
                      Sd.tt("dve", ya[:, half * 256:(half + 1) * 256].rearrange("p (h d) -> p h d", h=4), ov[:, :, 0:64],
                            rden[:, half * 4:half * 4 + 4].unsqueeze(2).to_broadcast([128, 4, 64]), ALU.mult,
                            [("pO", half), ("rden", half)], ["ya"])
                  ph, phk = pbfhalf()
                  for c in range(4):
                      Sd.tr(ph[:, c * 128:(c + 1) * 128], ya[:, c * 128:(c + 1) * 128], ident_b, ["ya", "cbf"], phk)
                  Sd.copy("act", yaT[:, :, tsl], ph.rearrange("p (c t) -> p c t", c=4), phk, ["yaT"])
                  for c in range(4):
                      pt, pk = pbank()
                      for hh in range(2):
                          g = c * 2 + hh
                          o = pt[hh * 64:(hh + 1) * 64, 0:128]
                          Sd.mm(o, vln[:, tt_, g * 64:(g + 1) * 64], wsTm[:, g * 128:(g + 1) * 128], True, False,
                                [("vln", tt_), "wsTm"], pk)
                          Sd.mm(o, one128[:, :], bs128[:, g * 128:(g + 1) * 128], False, True,
                                ["one128", "bs128"], pk)
                      sp_, spk = rhslot()
                      Sd.copy("act", sp_[:, 0:128], pt[:, 0:128], pk, [spk])
                      Sd.tt("dve", ybT[:, c, tsl], sp_[:, 0:128], uT[:, c, tsl], ALU.mult, [spk, "uT"], ["ybT"])
              if l == 0 and b == 0:
                  dump("yaT", yaT[:], [128, 4, TB], BF16, ["yaT"])
                  dump("ybT", ybT[:], [128, 4, TB], BF16, ["ybT"])
              ckpt("p7")

              wpav = wpa_d[l].rearrange("(kc p) f -> p kc f", p=128)
              wpbv = wpb_d[l].rearrange("(kc p) f -> p kc f", p=128)
              for gh in range(2):
                  wga, wgak = load_w(l, "ga%d" % gh)
                  wpa, wpak = load_w(l, "pa%d" % gh)
                  for c in range(4):
                      fo = gh * 4 + c
                      pt, pk = proj_fm(wga, wgak, c * 128, 128, 8, hrhs, ["hT"])
                      sa, sak = fscr()
                      Sd.act(sa[:], pt, AF.Sigmoid, pk, [sak])
                      pa, pak = proj_fm(wpa, wpak, c * 128, 128, 4, lambda kc: yaT[:, kc, :], ["yaT"])
                      Sd.tt("dve", mergedT[:, fo, :], pa, sa[:], ALU.mult, pak + [sak], [("mergedT", fo)])
                  wgb, wgbk = load_w(l, "gb%d" % gh)
                  wpb, wpbk = load_w(l, "pb%d" % gh)
                  for c in range(4):
                      fo = gh * 4 + c
                      pt, pk = proj_fm(wgb, wgbk, c * 128, 128, 8, hrhs, ["hT"])
                      sbt, sbk = fscr()
                      Sd.act(sbt[:], pt, AF.Sigmoid, pk, [sbk])
                      pb_, pbk = proj_fm(wpb, wpbk, c * 128, 128, 4, lambda kc: ybT[:, kc, :], ["ybT"])
                      m2, m2k = fscr()
                      Sd.tt("dve", m2[:], pb_, sbt[:], ALU.mult, pbk + [sbk], [m2k])
                      Sd.tt("pool", mergedT[:, fo, :], m2[:], mergedT[:, fo, :], ALU.add, [m2k, ("mergedT", fo)], [("mergedT", fo)])
              for oh in range(2):
                  wo, wok = load_w(l, "wo%d" % oh)
                  for c in range(4):
                      fo = oh * 4 + c
                      pt, pk = proj_fm(wo, wok, c * 128, 128, 8, lambda kc: mergedT[:, kc, :], [("mergedT", k_) for k_ in range(8)])
                      Sd.tt("dve", xT[:, fo, blk], pt, xT[:, fo, blk], ALU.add, pk + [("xT", b)], [("xT", b)])
              if l == 0 and b == 0:
                  dump("x1", xT[:, :, 0:TB], [128, 8, TB], F32, [("xT", 0)])
              ckpt("p9")

              rmsnorm_block(b, 16 + l * 8, hT, "hT")
              for j in range(6):
                  ncol = 512 if j < 5 else 256
                  wgv, wgk = load_w(l, "wg%d" % j)
                  wuv_, wuk_ = load_w(l, "wu%d" % j)
                  for c in range(ncol // 128):
                      fc = j * 4 + c
                      pg, pgk = proj_fm(wgv, wgk, c * 128, 128, 8, hrhs, ["hT"])
                      pu, puk = proj_fm(wuv_, wuk_, c * 128, 128, 8, hrhs, ["hT"])
                      sg, sgk = fscr()
                      Sd.act(sg[:], pg, AF.Silu, pgk, [sgk])
                      Sd.tt("dve", hid[:, fc, :], pu, sg[:], ALU.mult, puk + [sgk], [("hid", fc)])
              wdv = wd_d[l].rearrange("(fc p) d -> p fc d", p=128)
              for fo in range(8):
                  view, wk = load_w(l, "wd%d" % fo)
                  pt, pk = pbank()
                  for fc in range(NFC):
                      Sd.mm(pt, view[:, fc, :], hid[:, fc, :], fc == 0, fc == NFC - 1, [wk, ("hid", fc)], pk)
                  Sd.tt("dve", xT[:, fo, blk], pt, xT[:, fo, blk], ALU.add, pk + [("xT", b)], [("xT", b)])
              if l == 0 and b == 0:
                  dump("x2", xT[:, :, 0:TB], [128, 8, TB], F32, [("xT", 0)])
              ckpt("p10")
              ckpt("b%d" % b)


    except _Stop:
        pass

    final_keys = []
    onorm = fs
    oT = hidflat[:, 0:8192].bitcast(F32).rearrange("p (k t) -> p k t", k=8)
    ost = [hidflat[:, 8192:10240].bitcast(F32)]
    for b in range(NB):
        hidkeys = [("hid", fc) for fc in range(NFC)]
        blk = slice(b * TB, (b + 1) * TB)
        pt, pk = pbank()
        for kc in range(8):
            sq, sqk = rhslot()
            Sd.act(sq[:], xT[:, kc, blk], AF.Square, [("xT", b)], [sqk])
            Sd.mm(pt, ones_b, sq[:], kc == 0, kc == 7, [sqk, "cbf"], pk)
        ms, msk = fscr()
        Sd.ts("dve", ms[:], pt, 1.0 / D, 1e-6, ALU.mult, ALU.add, pk, [msk])
        rB, rk = fscr()
        Sd.tt("pool", rB[:], ms[:], small[:, 0:1].to_broadcast([128, TB]), ALU.pow, [msk, "small"], [rk])
        for kc in range(8):
            Sd.stt("dve", oT[:, kc, :], xT[:, kc, blk], gains[:, 32 + kc:33 + kc], rB[:],
                   ALU.mult, ALU.mult, [("xT", b), rk, "gains"] , hidkeys)
        for tt_ in range(4):
            i = b * 4 + tt_
            st = ost[0]
            for half in range(2):
                pt, pk = pbank()
                for c in range(4):
                    kc = half * 4 + c
                    Sd.tr(pt[:, c * 128:(c + 1) * 128], oT[:, kc, tt_ * 128:(tt_ + 1) * 128], ident_f, hidkeys + ["cf32"], pk)
                Sd.copy("dve" if half == 0 else "act", st[:, half * 512:(half + 1) * 512], pt, pk, ["ost"])
            Sd.dma("sp", out_d[i * 128:(i + 1) * 128, :], st, ["ost"], [("out", i)])
            final_keys.append(("out", i))
    for name, (tn, key) in dbg_outs.items():
        final_keys.append(key)

    Sd.emit(stack, final_keys)
    stack.close()
    return nc, Sd, dbg_outs


def _consts():
    ident = np.eye(128, dtype=np.float32)
    t = np.arange(128)
    tri_neg = np.where(t[None, :] <= t[:, None], 0.0, NEG).astype(np.float32)
    ones = np.ones((128, 128), np.float32)
    cf32 = np.concatenate([ident, tri_neg, ones], axis=1)
    triT = (t[:, None] <= t[None, :]).astype(np.float32)
    R = np.zeros((128, 128), np.float32)
    for base in (0, 64):
        for d in range(8):
            R[base + d + 8, base + d] = -1.0
            R[base + d, base + d + 8] = 1.0
    E = np.zeros((128, 128), np.float32)
    for d in range(16):
        E[d, d] = 1.0
    cbf = np.concatenate([ident, triT, R, E, ones], axis=1).astype(ml_dtypes.bfloat16)
    return cf32, cbf


def prep_shared(inputs):
    f = lambda k: np.asarray(inputs[k], dtype=np.float32)
    w_in = f("w_in")
    o_q, o_ckv, o_kr, o_qi, o_ki, o_wi, o_uv, o_g = 0, 512, 640, 656, 1168, 1232, 1240, 2264
    cols = np.concatenate([
        np.arange(o_q, o_q + 512),
        np.arange(o_qi, o_qi + 512),
        np.arange(o_ki, o_ki + 64), np.arange(o_ki, o_ki + 64),
        np.arange(o_ckv, o_ckv + 128),
        np.arange(o_kr, o_kr + 16),
        np.arange(o_uv, o_uv + 512),
        np.arange(o_g, o_g + 2048),
        np.arange(o_uv + 512, o_uv + 1024),
        np.arange(o_wi, o_wi + 8),
    ])
    assert cols.shape[0] == WIN_COLS
    w_in_r = np.ascontiguousarray(w_in[:, :, cols])
    w_uk = f("w_uk")
    w_ukT = np.zeros((L, 128, 8, 64), np.float32)
    w_ukT[:, :, :, 16:64] = np.transpose(w_uk, (0, 3, 1, 2))
    w_uvr = np.ascontiguousarray(np.transpose(f("w_uv"), (0, 2, 1, 3))).reshape(L, 128, 512)
    w_sT = np.ascontiguousarray(np.transpose(f("w_s"), (0, 3, 1, 2))).reshape(L, 128, 1024)
    b_sr = f("b_s").reshape(L, 1, 1024)
    gains = np.zeros((128, 80), np.float32)
    for k in range(32):
        gains[:, 48 + k] = 2.0 ** (-(k + 1))
    nm, nf, fn, kvn = f("norm_mix"), f("norm_ffn"), f("final_norm"), f("kv_norm")
    for l in range(L):
        gains[:, l * 8:(l + 1) * 8] = nm[l].reshape(8, 128).T
        gains[:, 16 + l * 8:16 + (l + 1) * 8] = nf[l].reshape(8, 128).T
        gains[:, 40 + l] = kvn[l]
    gains[:, 32:40] = fn.reshape(8, 128).T
    inv = (THETA ** (-np.arange(0, 16, 2, dtype=np.float32) / 16)).astype(np.float32)
    for base in (0, 64):
        for d in range(16):
            gains[base + d, 42] = inv[d % 8]
    cf32, cbf = _consts()
    return {
        "w_in_r": w_in_r, "w_ukT": w_ukT.reshape(L, 128, 512), "w_uvr": w_uvr, "w_sT": w_sT, "b_sr": b_sr,
        "ln_g": f("ln_v_g").reshape(L, 1, 512), "ln_b": f("ln_v_b").reshape(L, 1, 512),
        "w_pa": f("w_proj_a"), "w_pb": f("w_proj_b"), "w_out": f("w_out"),
        "w_gate": f("w_gate"), "w_up": f("w_up"), "w_down": f("w_down"),
        "gains": gains, "cf32": cf32, "cbf": cbf,
    }


_CACHE = {}


def kernel(**inputs):
    shared = prep_shared(inputs)
    x = np.asarray(inputs["x"], dtype=np.float32)
    pos = np.asarray(inputs["positions"]).astype(np.int32)
    if "nc" not in _CACHE:
        _CACHE["nc"] = build_nc()[0]
    nc = _CACHE["nc"]
    in_maps = []
    for c in range(8):
        m = dict(shared)
        m["x"] = np.ascontiguousarray(x[c])
        m["pos"] = np.ascontiguousarray(pos[c].reshape(1, S))
        in_maps.append(m)
    res = run_bass_kernel_spmd(nc, in_maps, core_ids=list(range(8)))
    out = np.stack([np.asarray(r["out"], dtype=np.float32) for r in res.results], axis=0)
    return out
```
